# Optimizing a Trainium2 kernel written in Bass

```python
import jax, jax.numpy as jnp
from jax import lax
import numpy as np

D_MODEL = 1024
BATCH = 16
SEQ = 4096
DEPTH = 2

MIX_WIDTH = 1024
SWA_HEADS = 8
SWA_KV_HEADS = 2
SWA_HEAD_DIM = 64
SWA_GROUP = SWA_HEADS // SWA_KV_HEADS
SWA_WIDTH = SWA_HEADS * SWA_HEAD_DIM
SWA_KV_WIDTH = SWA_KV_HEADS * SWA_HEAD_DIM
WINDOW = 128
DN_HEADS = 4
DN_HEAD_DIM = 64
DN_WIDTH = DN_HEADS * DN_HEAD_DIM
DN_CONV = 4
DN_CHUNK = 64
N_MEM = 256
MEM_HEADS = 4
MEM_HEAD_DIM = 64
MEM_WIDTH = MEM_HEADS * MEM_HEAD_DIM

EPS = 1e-6
IN_SIZES = (SWA_WIDTH, SWA_KV_WIDTH, SWA_KV_WIDTH,
            DN_WIDTH, DN_WIDTH, DN_WIDTH, DN_HEADS, DN_HEADS,
            MEM_WIDTH, MIX_WIDTH)
IN_WIDTH = SWA_WIDTH + 2 * SWA_KV_WIDTH + 3 * DN_WIDTH + 2 * DN_HEADS + MEM_WIDTH + MIX_WIDTH

kernel_name = "hybrid_swa_sink_gdn_memory_parallel_heads"


def _split_points(sizes):
    pts, acc = [], 0
    for s in sizes[:-1]:
        acc += s
        pts.append(acc)
    return pts


def rms_norm(x, g):
    xf = x.astype(jnp.float32)
    y = xf * lax.rsqrt(jnp.mean(xf * xf, axis=-1, keepdims=True) + EPS)
    return (y * g.astype(jnp.float32)).astype(x.dtype)


def l2_norm(x):
    xf = x.astype(jnp.float32)
    return xf * lax.rsqrt(jnp.sum(xf * xf, axis=-1, keepdims=True) + EPS)


def swa_sink_attention(q, k, v, sinks):
    B, S = q.shape[0], q.shape[1]
    nb = S // WINDOW
    qb = q.reshape(B, nb, WINDOW, SWA_KV_HEADS, SWA_GROUP, SWA_HEAD_DIM)
    kb = k.reshape(B, nb, WINDOW, SWA_KV_HEADS, SWA_HEAD_DIM)
    vb = v.reshape(B, nb, WINDOW, SWA_KV_HEADS, SWA_HEAD_DIM)

    def with_prev(t):
        prev = jnp.pad(t[:, :-1], ((0, 0), (1, 0), (0, 0), (0, 0), (0, 0)))
        return jnp.concatenate([prev, t], axis=2)

    kk, vv = with_prev(kb), with_prev(vb)
    scale = SWA_HEAD_DIM ** -0.5
    s = jnp.einsum("bnqhgd,bnkhd->bnhgqk", qb, kk).astype(jnp.float32) * scale
    qi = jnp.arange(WINDOW)[:, None]
    kj = jnp.arange(2 * WINDOW)[None, :]
    band = (kj > qi) & (kj <= qi + WINDOW)
    blk = jnp.arange(nb)[:, None, None]
    valid = band[None] & ((kj[None] >= WINDOW) | (blk > 0))
    s = jnp.where(valid[None, :, None, None], s, -jnp.inf)
    sink = sinks.astype(jnp.float32).reshape(1, 1, SWA_KV_HEADS, SWA_GROUP, 1, 1)
    m = jnp.maximum(jnp.max(s, axis=-1, keepdims=True), sink)
    p = jnp.exp(s - m)
    denom = jnp.sum(p, axis=-1, keepdims=True) + jnp.exp(sink - m)
    o = jnp.einsum("bnhgqk,bnkhd->bnqhgd", (p / denom).astype(v.dtype), vv)
    return o.reshape(B, S, SWA_WIDTH)


def causal_depthwise_conv(x, w):
    K, C = w.shape
    return lax.conv_general_dilated(
        x, w[:, None, :].astype(x.dtype), window_strides=(1,), padding=[(K - 1, 0)],
        dimension_numbers=("NWC", "WIO", "NWC"), feature_group_count=C)


def gated_delta_rule(q, k, v, g, beta):
    B, S, H, dk = q.shape
    dv = v.shape[-1]
    C = DN_CHUNK
    N = S // C

    def chunks(t):
        return t.reshape(B, N, C, H, -1).transpose(0, 3, 1, 2, 4)

    q = chunks(q) * (dk ** -0.5)
    k = chunks(k)
    v = chunks(v)
    g = jnp.cumsum(g.reshape(B, N, C, H).transpose(0, 3, 1, 2), axis=-1)
    beta = beta.reshape(B, N, C, H).transpose(0, 3, 1, 2)
    causal = jnp.tril(jnp.ones((C, C), dtype=bool))
    strict = jnp.tril(jnp.ones((C, C), dtype=bool), -1)
    decay = jnp.exp(jnp.where(causal, g[..., :, None] - g[..., None, :], -jnp.inf))
    k_beta = k * beta[..., None]
    v_beta = v * beta[..., None]
    L = jnp.where(strict, jnp.einsum("bhncd,bhnsd->bhncs", k_beta, k) * decay, 0.0)
    a = L + jnp.eye(C, dtype=jnp.float32)
    rhs = jnp.concatenate([v_beta, k_beta * jnp.exp(g)[..., None]], axis=-1)
    sol = lax.linalg.triangular_solve(a, rhs, left_side=True, lower=True, unit_diagonal=True)
    u, w = sol[..., :dv], sol[..., dv:]

    def step(state, inp):
        q_i, k_i, u_i, w_i, g_i, dec_i = inp
        v_new = u_i - jnp.einsum("bhck,bhkv->bhcv", w_i, state)
        intra = jnp.einsum("bhck,bhsk->bhcs", q_i, k_i) * dec_i
        o_i = (jnp.einsum("bhck,bhkv->bhcv", q_i * jnp.exp(g_i)[..., None], state)
               + jnp.einsum("bhcs,bhsv->bhcv", intra, v_new))
        g_last = g_i[..., -1:]
        state = (state * jnp.exp(g_last)[..., None]
                 + jnp.einsum("bhck,bhcv->bhkv", k_i * jnp.exp(g_last - g_i)[..., None], v_new))
        return state, o_i

    xs = tuple(jnp.moveaxis(t, 2, 0) for t in (q, k, u, w, g, decay))
    s0 = jnp.zeros((B, H, dk, dv), jnp.float32)
    _, o = lax.scan(step, s0, xs)
    return o.transpose(1, 0, 3, 2, 4).reshape(B, S, H, dv)


def memory_attention(q, mk, mv):
    s = jnp.einsum("bshd,bmhd->bhsm", q, mk).astype(jnp.float32) * (MEM_HEAD_DIM ** -0.5)
    p = jax.nn.softmax(s, axis=-1).astype(mv.dtype)
    return jnp.einsum("bhsm,bmhd->bshd", p, mv)


def hybrid_layer(x, mem, pre_g, w_in, conv_w, a_log, dt_bias, sinks, dn_norm_g,
                 mem_norm_g, w_mem_kv, w_out, post_g):
    B, S, _ = x.shape
    h = rms_norm(x, pre_g)
    proj = h @ w_in
    sq, sk, sv, dq, dk, dv, dbeta, dalpha, mq, gate = jnp.split(
        proj, _split_points(IN_SIZES), axis=-1)

    a_out = swa_sink_attention(
        sq.reshape(B, S, SWA_HEADS, SWA_HEAD_DIM),
        sk.reshape(B, S, SWA_KV_HEADS, SWA_HEAD_DIM),
        sv.reshape(B, S, SWA_KV_HEADS, SWA_HEAD_DIM), sinks)

    qkv = jax.nn.silu(causal_depthwise_conv(jnp.concatenate([dq, dk, dv], axis=-1), conv_w))
    cq, ck, cv = jnp.split(qkv, [DN_WIDTH, 2 * DN_WIDTH], axis=-1)
    qf = l2_norm(cq.reshape(B, S, DN_HEADS, DN_HEAD_DIM))
    kf = l2_norm(ck.reshape(B, S, DN_HEADS, DN_HEAD_DIM))
    vf = cv.reshape(B, S, DN_HEADS, DN_HEAD_DIM).astype(jnp.float32)
    beta = jax.nn.sigmoid(dbeta.astype(jnp.float32))
    g = -jnp.exp(a_log.astype(jnp.float32)) * jax.nn.softplus(
        dalpha.astype(jnp.float32) + dt_bias.astype(jnp.float32))
    d_out = gated_delta_rule(qf, kf, vf, g, beta)
    d_out = rms_norm(d_out, dn_norm_g).reshape(B, S, DN_WIDTH).astype(x.dtype)

    mkv = rms_norm(mem, mem_norm_g) @ w_mem_kv
    mk, mv = jnp.split(mkv, [MEM_WIDTH], axis=-1)
    Mn = mem.shape[1]
    m_out = memory_attention(
        mq.reshape(B, S, MEM_HEADS, MEM_HEAD_DIM),
        mk.reshape(B, Mn, MEM_HEADS, MEM_HEAD_DIM),
        mv.reshape(B, Mn, MEM_HEADS, MEM_HEAD_DIM)).reshape(B, S, MEM_WIDTH)

    mixed = jnp.concatenate([a_out, d_out, m_out], axis=-1) * jax.nn.silu(gate)
    y = mixed @ w_out
    return x + rms_norm(y, post_g)


def setup_inputs(seed: int = 0) -> dict:
    key = jax.random.key(seed)
    ks = jax.random.split(key, 16)
    f32 = jnp.float32
    x = jax.random.normal(ks[0], (BATCH, SEQ, D_MODEL), f32)
    mem = jax.random.normal(ks[1], (BATCH, N_MEM, D_MODEL), f32)
    pre_norm_g = 1.0 + 0.05 * jax.random.normal(ks[2], (DEPTH, D_MODEL), f32)
    w_in = jax.random.normal(ks[3], (DEPTH, D_MODEL, IN_WIDTH), f32) * D_MODEL ** -0.5
    conv_w = jax.random.normal(ks[4], (DEPTH, DN_CONV, 3 * DN_WIDTH), f32) * DN_CONV ** -0.5
    a_log = jnp.log(jax.random.uniform(ks[5], (DEPTH, DN_HEADS), f32, 1.0, 16.0))
    dt = jnp.exp(jax.random.uniform(ks[6], (DEPTH, DN_HEADS), f32,
                                    float(np.log(1e-3)), float(np.log(1e-1))))
    dt_bias = dt + jnp.log(-jnp.expm1(-dt))
    sinks = 0.5 * jax.random.normal(ks[7], (DEPTH, SWA_HEADS), f32)
    dn_norm_g = 1.0 + 0.05 * jax.random.normal(ks[8], (DEPTH, DN_HEAD_DIM), f32)
    mem_norm_g = 1.0 + 0.05 * jax.random.normal(ks[9], (DEPTH, D_MODEL), f32)
    w_mem_kv = jax.random.normal(ks[10], (DEPTH, D_MODEL, 2 * MEM_WIDTH), f32) * D_MODEL ** -0.5
    w_out = jax.random.normal(ks[11], (DEPTH, MIX_WIDTH, D_MODEL), f32) * MIX_WIDTH ** -0.5
    post_norm_g = 1.0 + 0.05 * jax.random.normal(ks[12], (DEPTH, D_MODEL), f32)
    return {"x": x, "mem": mem, "pre_norm_g": pre_norm_g, "w_in": w_in, "conv_w": conv_w,
            "a_log": a_log, "dt_bias": dt_bias, "sinks": sinks, "dn_norm_g": dn_norm_g,
            "mem_norm_g": mem_norm_g, "w_mem_kv": w_mem_kv, "w_out": w_out,
            "post_norm_g": post_norm_g}


def reference(x, mem, pre_norm_g, w_in, conv_w, a_log, dt_bias, sinks, dn_norm_g,
              mem_norm_g, w_mem_kv, w_out, post_norm_g):
    for l in range(DEPTH):
        x = hybrid_layer(x, mem, pre_norm_g[l], w_in[l], conv_w[l], a_log[l], dt_bias[l],
                         sinks[l], dn_norm_g[l], mem_norm_g[l], w_mem_kv[l], w_out[l],
                         post_norm_g[l])
    return x
```

```python
import contextlib
import numpy as np
import concourse.bass as bass
import concourse.mybir as mybir
from concourse.bass_utils import run_bass_kernel_spmd

F32 = mybir.dt.float32
BF16 = mybir.dt.bfloat16
AF = mybir.ActivationFunctionType
ALU = mybir.AluOpType
AX = mybir.AxisListType

D = 1024
NCORES = 8
EPS = 1e-6
EPOCH = 12000
IL_RATIO = 1
O_SQ, O_SK, O_SV, O_DQ, O_DK, O_DV, O_BETA, O_ALPHA, O_MQ, O_GATE = 0, 512, 640, 768, 1024, 1280, 1536, 1540, 1544, 1800
NF = 13
WFC = NF * 128
WTC = 128 + 8 + 1024
WALLC = WFC + WTC + 1024
PV_GPRE, PV_GMEM, PV_CONV = 0, 8, 16
NPV = 40
RB_POST, RB_DNG, RB_SINK, RB_ALOG, RB_DTB = 0, 1024, 1088, 1096, 1100
NRB = 1104
C_ID, C_SL, C_UL, C_BONES, C_UTB, C_HM, C_MPREV, C_MCUR = 0, 128, 256, 384, 512, 640, 644, 772
NCST = 644
NCONST = 900


class Sem:
    def __init__(self, h, owner):
        self.h = h
        self.owner = owner
        self.count = 0


class Buf:
    __slots__ = ("name", "w", "r")

    def __init__(self, name):
        self.name = name
        self.w = None
        self.r = {}


class Eng:
    def __init__(self, name, self_sync):
        self.name = name
        self.self_sync = self_sync
        self.sem = None
        self.known = {}
        self.prog = []


class KB:
    def __init__(self, nc, stack, self_sync=True):
        self.nc = nc
        self.stack = stack
        self.nsem = 0
        self.engs = {n: Eng(n, ss) for n, ss in
                     (("pe", False), ("dve", self_sync), ("act", self_sync), ("pool", self_sync), ("sp", False))}
        for e in self.engs.values():
            e.sem = self.new_sem(e.name)

    def new_sem(self, owner):
        h = self.stack.enter_context(self.nc.semaphore(f"s{self.nsem}_{owner}"))
        self.nsem += 1
        return Sem(h, owner)

    def _waits(self, E, reads, writes):
        need = {}

        def req(sv):
            if sv is None:
                return
            s, v = sv
            if s.owner == E.name and not E.self_sync:
                return
            if need.get(s, 0) < v:
                need[s] = v
        for b in reads:
            req(b.w)
        for b in writes:
            req(b.w)
            for s, v in b.r.items():
                req((s, v))
        wl = []
        for s, v in need.items():
            if E.known.get(s, 0) < v:
                E.known[s] = v
                wl.append((s.h, v))
        return wl

    def op(self, e, fn, reads=(), writes=()):
        E = self.engs[e]
        wl = self._waits(E, reads, writes)
        if E.sem.count >= EPOCH:
            E.sem = self.new_sem(E.name)
        sem = E.sem
        sem.count += 1
        val = sem.count
        E.prog.append((wl, fn, sem.h, 1))
        for b in reads:
            b.r[sem] = val
        for b in writes:
            b.w = (sem, val)
            b.r = {}

    def dma(self, e, fn, dsem, reads=(), writes=()):
        E = self.engs[e]
        wl = self._waits(E, reads, writes)
        dsem.count += 16
        val = dsem.count
        E.prog.append((wl, fn, dsem.h, 16))
        for b in reads:
            b.r[dsem] = val
        for b in writes:
            b.w = (dsem, val)
            b.r = {}

    def barrier(self):
        for E in self.engs.values():
            wl = []
            for O in self.engs.values():
                if O is E:
                    continue
                if O.sem.count > 0 and E.known.get(O.sem, 0) < O.sem.count:
                    E.known[O.sem] = O.sem.count
                    wl.append((O.sem.h, O.sem.count))
            if wl:
                E.prog.append((wl, None, None, 0))

    def final_wait(self, e, sems):
        E = self.engs[e]
        wl = [(s.h, s.count) for s in sems if s.count > 0]
        E.prog.append((wl, None, None, 0))

    def emit(self):
        nc = self.nc
        progs = {n: E.prog for n, E in self.engs.items()}

        def run(eng, prog):
            for wl, fn, semh, inc in prog:
                for h, v in wl:
                    eng.wait_ge(h, v)
                if fn is not None:
                    fn(eng).then_inc(semh, inc)

        with nc.Block() as block:
            @block.tensor
            def _(eng):
                run(eng, progs["pe"])

            @block.vector
            def _(eng):
                run(eng, progs["dve"])

            @block.scalar
            def _(eng):
                run(eng, progs["act"])

            @block.gpsimd
            def _(eng):
                run(eng, progs["pool"])

            @block.sync
            def _(eng):
                run(eng, progs["sp"])


def build(NSEQ, S, DEPTH, dbg_names=(), self_sync=True):
    NBLK = S // 128
    nc = bass.Bass("TRN2", target_bir_lowering=False)
    dt = nc.dram_tensor
    x_d = dt("x", [NSEQ * S, D], F32, kind="ExternalInput").ap()
    mem_d = dt("mem", [NSEQ * 256, D], F32, kind="ExternalInput").ap()
    wall_d = dt("wall", [DEPTH, 128, 8, WALLC], F32, kind="ExternalInput").ap()
    wkv_d = dt("wkv", [DEPTH, 128, 8, 512], F32, kind="ExternalInput").ap()
    pvec_d = dt("pvec", [128, DEPTH, NPV], F32, kind="ExternalInput").ap()
    rowb_d = dt("rowb", [128, DEPTH, NRB], F32, kind="ExternalInput").ap()
    const_d = dt("consts", [128, NCONST], F32, kind="ExternalInput").ap()
    out_d = dt("out", [NSEQ * S, D], F32, kind="ExternalOutput").ap()
    dbg_d = {}

    with contextlib.ExitStack() as st:
        K = KB(nc, st, self_sync)

        def sb(name, shape, dtype):
            return st.enter_context(nc.sbuf_tensor(name, shape, dtype))

        Wf = sb("Wf", [128, DEPTH, 8, WFC], BF16)
        Wt = sb("Wt", [128, DEPTH, 8, WTC], BF16)
        Wo = sb("Wo", [128, DEPTH, 8, 1024], BF16)
        pvec = sb("pvec_s", [128, DEPTH, NPV], F32)
        rowb = sb("rowb_s", [128, DEPTH, NRB], F32)
        cst = sb("cst", [128, NCST], F32)
        onesf = sb("onesf", [128, 128], F32)
        idb = sb("idb", [128, 128], BF16)
        swam = sb("swam", [128, 2, 128], BF16)
        lay = sb("lay", [128, DEPTH, 16], F32)
        scratch = sb("scratch", [128, 4096], BF16)
        wkv = scratch[:, :].rearrange("p (k c) -> p k c", k=8)
        pexp = scratch[:, 0:1024]
        mixed1 = sb("mixed1", [128, 1024], BF16)
        sgs = [scratch[:, 1024:2048], scratch[:, 2048:3072]]
        mixeds = [scratch[:, 3072:4096], mixed1[:, :]]
        KF = sb("KF", [128, NSEQ, DEPTH, 2, 128], BF16)
        Vc = sb("Vc", [128, NSEQ, DEPTH, 2, 2, 65], BF16)
        DFtail = sb("DFtail", [128, NSEQ, DEPTH, 6, 3], F32)
        DFw = sb("DFw", [128, 6, 131], F32)
        S32 = sb("S32", [128, NSEQ, DEPTH, 2, 64], F32)
        Sbf = sb("Sbf", [128, NSEQ, DEPTH, 2, 64], BF16)
        mkT = sb("mkT", [128, DEPTH, NSEQ, 2, 256], BF16)
        mv = sb("mv", [128, DEPTH, NSEQ, 2, 4, 65], BF16)
        xs = sb("xs", [128, 2, D], F32)
        tT = sb("tT", [128, 2, 8, 128], BF16)
        QM = sb("QM", [128, 12, 128], BF16)
        QFz = QM[:, 0:8, :]
        MFz = QM[:, 8:12, :]
        mixT = QM[:, 0:8, :]
        cs = sb("cs", [128, 6, 128], F32)
        sq = sb("sq", [128, 4, 128], F32)
        qkn = sb("qkn", [128, 4, 128], BF16)
        vT = sb("vT", [128, 2, 128], BF16)
        kTz = sb("kTz", [128, 2, 2, 128], BF16)
        qg = sb("qg", [128, 2, 128], BF16)
        qgz = sb("qgz", [128, 2, 2, 128], BF16)
        eG = sb("eG", [128, 2, 128], F32)
        kvtok = sb("kvtok", [128, 2, 4, 64], BF16)
        kdz = sb("kdz", [128, 2, 4, 64], BF16)
        sc = sb("sc", [128, 64], F32)
        bonesb = sb("bonesb", [128, 128], BF16)
        gs = sb("gs", [128, 4, 512], F32)
        Aa = sb("Aa", [128, 2, 512], BF16)
        Bb = sb("Bb", [128, 2, 512], BF16)
        Pp = sb("Pp", [128, 2, 512], BF16)
        gb = sb("gb", [128, 3, 512], BF16)
        ksv = sb("ksv", [128, 2, 256], BF16)
        psum = st.enter_context(nc.psum_tensor("psum", [128, 4096], F32))

        hTs = [tT[:, 0], tT[:, 0]]
        sqb = tT[:, 1].rearrange("p k t -> p (k t)")[:, 0:512]
        hbf0 = gs[:, 0, :].bitcast(BF16)
        Dd = gs[:, 0, :]
        Dt = gs[:, 1, :]
        uu = gs[:, 2, 0:256]
        oo = gs[:, 2, 256:512]
        osq = gs[:, 3, 0:256]
        ytmp = gs[:, 0:2, :].rearrange("p a b -> p (a b)")
        E1 = Pp[:, 0, :]
        E2 = Pp[:, 1, :]
        Tb, Wp, inT = (gb[:, i, :] for i in range(3))
        kS = ksv[:, 0, :]
        vnew = ksv[:, 1, :]

        def pbank(i, n=1):
            return psum[:, i * 512:(i + n) * 512]
        pj = [pbank(0), pbank(1)]
        ptrf = pbank(2)
        ptr = pbank(2).bitcast(BF16)
        pst = pbank(3, 2)
        po = pbank(5)
        pgA = pbank(6)
        pgB = pbank(7)

        B = {}

        def bf(n):
            if n not in B:
                B[n] = Buf(n)
            return B[n]
        b_pj = [bf("pj0"), bf("pj1")]
        b_ptr, b_po, b_pgA, b_pgB = bf("ptr"), bf("po"), bf("pgA"), bf("pgB")
        b_pst = (bf("ps3"), bf("ps4"))
        b_xs = [bf("xs0"), bf("xs1")]
        b_Dd, b_Dt, b_uo, b_osq = bf("Dd"), bf("Dt"), bf("uo"), bf("osq")
        b_yt = (b_Dd, b_Dt)
        b_E1, b_E2 = bf("P0"), bf("P1")
        b_Tb, b_Wp, b_inT = bf("Tb"), bf("Wp"), bf("inT")
        b_kS, b_vnew = bf("kS"), bf("vnew")
        b_A = [bf("A0"), bf("A1")]
        b_B = [bf("B0"), bf("B1")]
        b_P = [bf("P0"), bf("P1")]

        d_x = [K.new_sem("dx0"), K.new_sem("dx1")]
        d_o = [K.new_sem("do0"), K.new_sem("do1")]
        d_c = K.new_sem("dc")
        d_dbg = K.new_sem("ddbg")

        def MM(out, lhsT, rhs, reads, writes, start=True, stop=True):
            K.op("pe", lambda e: e.matmul(out, lhsT, rhs, start=start, stop=stop), reads, writes)

        def TR(out, in_, ident, reads, writes):
            K.op("pe", lambda e: e.transpose(out, in_, ident), reads, writes)

        def ACT(out, in_, func, reads, writes, bias=None, scale=None, accum=None):
            kw = {}
            if bias is not None:
                kw["bias"] = bias
            if scale is not None:
                kw["scale"] = scale
            if accum is not None:
                kw["accum_out"] = accum
            K.op("act", lambda e: e.activation(out, in_, func, **kw), reads, writes)

        def TT(eng, out, in0, in1, op, reads, writes):
            K.op(eng, lambda e: e.tensor_tensor(out, in0, in1, op), reads, writes)

        def TS(eng, out, in0, s1, s2, op0, op1, reads, writes):
            if op1 is None:
                K.op(eng, lambda e: e.tensor_scalar(out, in0, s1, None, op0), reads, writes)
            else:
                K.op(eng, lambda e: e.tensor_scalar(out, in0, s1, s2, op0, op1), reads, writes)

        def STT(out, in0, scalar, in1, op0, op1, reads, writes):
            K.op("dve", lambda e: e.scalar_tensor_tensor(out, in0, scalar, in1, op0, op1), reads, writes)

        def CP(eng, out, in_, reads, writes):
            if eng == "act":
                K.op("act", lambda e: e.copy(out, in_), reads, writes)
            else:
                K.op(eng, lambda e: e.tensor_copy(out, in_), reads, writes)

        def RECIP(out, in_, reads, writes):
            K.op("dve", lambda e: e.reciprocal(out, in_), reads, writes)

        def MEMSET(eng, ap, val, writes):
            K.op(eng, lambda e: e.memset(ap, val), (), writes)

        def DMA(out, in_, dsem, reads, writes):
            K.dma("sp", lambda e: e.dma_start(out=out, in_=in_), dsem, reads, writes)

        def dbg(name, ap, shape, reads, dtype=F32):
            if name not in dbg_names or name in dbg_d:
                return
            t = dt("dbg_" + name, list(shape), dtype, kind="ExternalOutput").ap()
            dbg_d[name] = t
            DMA(t, ap, d_dbg, reads, ())

        def bc(ap, shape):
            return ap.broadcast_to(list(shape))

        def v3(ap, a):
            return ap.rearrange("p (a b) -> p a b", a=a)

        b_c = bf("consts")
        DMA(cst[:, :], const_d[:, 0:NCST], d_c, (), (b_c,))
        DMA(xs[:, 0, 0:256], const_d[:, C_MPREV:C_MPREV + 256], d_c, (), (b_c,))
        DMA(pvec[:, :, :], pvec_d, d_c, (), (b_c,))
        DMA(rowb[:, :, :], rowb_d, d_c, (), (b_c,))
        b_c.w = (d_c, d_c.count)
        b_c2 = bf("consts2")
        CP("dve", idb[:, :], cst[:, C_ID:C_ID + 128], (b_c,), (b_c2,))
        CP("dve", swam[:, :, :], v3(xs[:, 0, 0:256], 2), (b_c,), (b_c2, b_xs[0]))
        MEMSET("pool", onesf[:, :], 1.0, (b_c2,))
        CP("dve", bonesb[:, :], cst[:, C_BONES:C_BONES + 128], (b_c,), (b_c2,))
        for l in range(DEPTH):
            ACT(lay[:, l, 0:8], rowb[:, l, RB_SINK:RB_SINK + 8], AF.Exp, (b_c,), (b_c2,))
            ACT(lay[:, l, 8:12], rowb[:, l, RB_ALOG:RB_ALOG + 4], AF.Exp, (b_c,), (b_c2,))
            TS("dve", lay[:, l, 8:12], lay[:, l, 8:12], -1.0, None, ALU.mult, None, (b_c2,), (b_c2,))
        b_st = bf("state")
        MEMSET("pool", Vc[:, :, :, :, :, 64:65], 1.0, (b_st,))
        MEMSET("pool", mv[:, :, :, :, :, 64:65], 1.0, (b_st,))
        MEMSET("pool", ksv[:, :, :], 0.0, (b_kS, b_vnew))
        ident = cst[:, C_ID:C_ID + 128]
        SLm = cst[:, C_SL:C_SL + 128]
        ULm = cst[:, C_UL:C_UL + 128]
        bones = cst[:, C_BONES:C_BONES + 128]
        utb = cst[:, C_UTB:C_UTB + 128]
        hm = cst[:, C_HM:C_HM + 2]

        b_W = bf("W")
        b_wkv = bf("wkv")
        b_hbf, b_sc, b_tT = bf("mixed"), bf("sc"), bf("tT")
        b_mk = bf("mk")
        npiece = 0
        for l in range(DEPTH):
            for kc in range(8):
                pieces = [(Wf, 0, 0, 1024, True), (Wf, 1024, 1024, WFC - 1024, True),
                          (Wt, 0, WFC, 1024, True), (Wt, 1024, WFC + 1024, WTC - 1024, True),
                          (Wo, 0, WFC + WTC, 1024, False)]
                for (dst, dcol, scol, n, gain) in pieces:
                    slot = npiece % 2
                    npiece += 1
                    DMA(xs[:, slot, 0:n], wall_d[l, :, kc, scol:scol + n], d_x[slot], (), (b_xs[slot],))
                    eng = "dve" if slot == 0 else "pool"
                    if gain:
                        TS(eng, dst[:, l, kc, dcol:dcol + n], xs[:, slot, 0:n],
                           pvec[:, l, PV_GPRE + kc:PV_GPRE + kc + 1], 0.0, ALU.mult, ALU.add,
                           (b_xs[slot], b_c), (b_W,))
                    else:
                        CP(eng, dst[:, l, kc, dcol:dcol + n], xs[:, slot, 0:n], (b_xs[slot],), (b_W,))
            for k2 in range(4):
                slot = npiece % 2
                npiece += 1
                DMA(v3(xs[:, slot, :], 2), wkv_d[l, :, 2 * k2:2 * k2 + 2, :], d_x[slot], (), (b_xs[slot],))
                for kk in range(2):
                    kc = 2 * k2 + kk
                    TS("dve", wkv[:, kc, :], xs[:, slot, kk * 512:(kk + 1) * 512],
                       pvec[:, l, PV_GMEM + kc:PV_GMEM + kc + 1], 0.0, ALU.mult, ALU.add,
                       (b_xs[slot], b_c), (b_wkv,))
            for s in range(NSEQ):
                for mb in range(2):
                    slot = npiece % 2
                    npiece += 1
                    r0 = s * 256 + mb * 128
                    DMA(xs[:, slot, :], mem_d[r0:r0 + 128, :], d_x[slot], (), (b_xs[slot],))
                    ACT(hbf0, xs[:, slot, :], AF.Square, (b_xs[slot],), (b_hbf, b_sc), accum=sc[:, 0:1])
                    ACT(sc[:, 1:2], sc[:, 0:1], AF.Sqrt, (b_sc,), (b_sc,), bias=EPS, scale=1.0 / D)
                    RECIP(sc[:, 2:3], sc[:, 1:2], (b_sc,), (b_sc,))
                    TS("dve", hbf0, xs[:, slot, :], sc[:, 2:3], None, ALU.mult, None,
                       (b_xs[slot], b_sc), (b_hbf,))
                    for kc in range(8):
                        TR(ptr[:, kc * 128:(kc + 1) * 128], hbf0[:, kc * 128:(kc + 1) * 128], idb[:, :],
                           (b_hbf, b_c2), (b_ptr,))
                    CP("act", tT[:, mb].rearrange("p k t -> p (k t)"), ptr[:, :], (), (b_ptr, b_tT))
                for pair in range(2):
                    for kc in range(8):
                        MM(pj[0][:, 0:256], wkv[:, kc, pair * 128:(pair + 1) * 128], tT[:, :, kc, :],
                           (b_wkv, b_tT), (b_pj[0],), start=(kc == 0), stop=(kc == 7))
                    CP("act", mkT[:, l, s, pair, :], pj[0][:, 0:256], (), (b_pj[0], b_mk))
                for mb in range(2):
                    for kc in range(8):
                        MM(pj[1][:, 0:256], tT[:, mb, kc, :], wkv[:, kc, 256:512],
                           (b_wkv, b_tT), (b_pj[1],), start=(kc == 0), stop=(kc == 7))
                    CP("dve", mv[:, l, s, mb, :, 0:64], v3(pj[1][:, 0:256], 4),
                       (b_st,), (b_pj[1], b_mk))
                dbg(f"mkT{l}", mkT[:, l, s], [128, 2, 256], (b_mk,), BF16)
                dbg(f"mv{l}", mv[:, l, s], [128, 2, 4, 65], (b_mk,), BF16)
        K.barrier()

        cP = [pbank(0), pbank(1)]
        cst_ = pbank(0, 2)
        cO = pbank(2)
        cT = pbank(3).bitcast(BF16)
        gA, gB, gC = pbank(4), pbank(5), pbank(6)
        gTf = pbank(7)
        gT = pbank(7).bitcast(BF16)
        b_cP = [bf("pj0"), bf("pj1")]
        b_cst = (b_cP[0], b_cP[1])
        b_cO, b_cT = bf("ptr"), bf("ps3")
        b_gA, b_gB, b_gC, b_gT = bf("ps4"), bf("po"), bf("pgA"), bf("pgB")
        b_mixA = [bf("mixA0"), bf("mixA1")]
        b_mixG = [bf("mixG0"), bf("mixG1")]
        b_sgs = [bf("sg0"), bf("sg1")]
        b_hTs = [bf("hT0"), bf("hT0")]
        b_QF, b_MF, b_V = bf("QF"), bf("MF"), bf("V")
        b_KFs = [bf("KF0"), bf("KF1")]
        b_DFw, b_DFt, b_cs, b_sq = bf("DFw"), bf("DFt"), bf("cs"), bf("sq")
        b_qkn, b_vT, b_kTz, b_eG, b_qg, b_qgz = (bf(n) for n in ("qkn", "vT", "kTz", "eG", "qg", "qgz"))
        b_kvtok, b_kdz, b_pexp = bf("kvtok"), bf("kdz"), bf("pexp")
        b_S32, b_Sbf = bf("S32"), bf("Sbf")
        b_scG, b_tmpv, b_csv, b_BA, b_sqb = bf("scG"), bf("tmpv"), bf("csv"), bf("BA"), bf("sqb")
        tmpv = gs[:, 3, 256:512]
        ytmp2 = pexp.bitcast(F32)
        SS, RT, RSTD, DEN, SSY = 0, 1, 2, 4, 8
        BA, BETA, GRAW, GTOK, EGT, BGE, SS4 = 0, 8, 12, 16, 20, 24, 28
        scA = sc[:, 0:16]
        scG = sc[:, 16:64]

        def rsqrt_act(out, in_, scale, reads, writes):
            ACT(out, in_, AF.Ln, reads, writes, bias=EPS, scale=scale)
            ACT(out, out, AF.Exp, (), writes, scale=-0.5)

        def chain(*gens):
            for g in gens:
                yield from g

        def interleave(g1, g2, r1, r2):
            d1 = d2 = False
            while not (d1 and d2):
                n2 = r2
                if not d1:
                    try:
                        v = next(g1)
                        n2 = r2 if v is None else v
                    except StopIteration:
                        d1 = True
                if d1:
                    n2 = 1000
                for _ in range(n2):
                    if not d2:
                        try:
                            next(g2)
                        except StopIteration:
                            d2 = True

        def projF(bankap, bbank, pos, fc, l, hT, b_hT):
            for kc in range(8):
                MM(bankap[:, pos * 128:(pos + 1) * 128], Wf[:, l, kc, fc * 128:(fc + 1) * 128], hT[:, kc, :],
                   (b_hT,), (bbank,), start=(kc == 0), stop=(kc == 7))

        def projFg(bankap, bbank, pos, fc, l, hT, b_hT):
            for kc in range(8):
                MM(bankap[:, pos * 128:(pos + 1) * 128], Wf[:, l, kc, fc * 128:(fc + 1) * 128], hT[:, kc, :],
                   (b_hT,), (bbank,), start=(kc == 0), stop=(kc == 7))
                if kc % 4 == 3:
                    yield

        qconv_done = [-1]

        def genP(s, b, l, k):
            slot = s
            bx = b_xs[slot]
            xt = xs[:, slot, :]
            hT, b_hT = hTs[s], b_hTs[s]
            sg, b_sg = sgs[s], b_sgs[s]
            mixed, bmA, bmG = mixeds[s], b_mixA[s], b_mixG[s]
            hbf = mixed
            cur, prv = b % 2, 1 - (b % 2)
            kbs = [1] if b == 0 else [0, 1]
            ring = {0: prv, 1: cur}
            if l == 0:
                i = s * NBLK + b
                DMA(xt, x_d[i * 128:(i + 1) * 128, :], d_x[slot], (), (bx,))
            ACT(hbf, xt, AF.Square, (bx,), (bmA, bmG, b_sc), accum=scA[:, SS:SS + 1])
            rsqrt_act(scA[:, RSTD:RSTD + 1], scA[:, SS:SS + 1], 1.0 / D, (), (b_sc,))
            yield
            TS("dve", hbf, xt, scA[:, RSTD:RSTD + 1], None, ALU.mult, None, (bx, b_sc), (bmA, bmG))
            yield
            for kc in range(8):
                TR(cT[:, kc * 128:(kc + 1) * 128], hbf[:, kc * 128:(kc + 1) * 128], idb[:, :], (bmA, bmG), (b_cT,))
            CP("act", hT.rearrange("p k t -> p (k t)"), cT[:, :], (), (b_cT, b_hT))
            yield
            for c in range(4):
                yield from projFg(cP[0], b_cP[0], c, c, l, hT, b_hT)
            for g2 in range(2):
                ACT(QFz[:, 4 * g2:4 * g2 + 4, :].rearrange("p c t -> p (c t)"), cP[0], AF.Copy, (b_c,),
                    (b_cP[0], b_QF), scale=hm[:, g2:g2 + 1])
            yield from projFg(cP[1], b_cP[1], 0, 4, l, hT, b_hT)
            yield from projFg(cP[1], b_cP[1], 1, 11, l, hT, b_hT)
            yield from projFg(cP[1], b_cP[1], 2, 12, l, hT, b_hT)
            CP("act", KF[:, s, l, cur, :], cP[1][:, 0:128], (), (b_cP[1], b_KFs[s]))
            for h2 in range(2):
                TS("dve", MFz.rearrange("p (a b) t -> p a b t", a=2)[:, :, h2, :], v3(cP[1][:, 128:384], 2),
                   hm[:, h2:h2 + 1], None, ALU.mult, None, (b_c,), (b_cP[1], b_MF))
            yield
            for kc in range(8):
                MM(cP[0][:, 0:128], hT[:, kc, :], Wt[:, l, kc, 0:128], (b_hT,), (b_cP[0],),
                   start=(kc == 0), stop=(kc == 7))
            CP("act", Vc[:, s, l, cur, :, 0:64], v3(cP[0][:, 0:128], 2), (b_st,), (b_cP[0], b_V))
            yield
            for half in range(2):
                bank = 1 - half
                hs = slice(half * 512, (half + 1) * 512)
                for kc in range(8):
                    MM(cP[bank], hT[:, kc, :], Wt[:, l, kc, 136 + half * 512:136 + (half + 1) * 512],
                       (b_hT,), (b_cP[bank],), start=(kc == 0), stop=(kc == 7))
                    yield
                ACT(sg[:, hs], cP[bank], AF.Exp, (), (b_cP[bank], b_sg), scale=-1.0)
                yield
                ACT(sg[:, hs], sg[:, hs], AF.Ln, (), (b_sg,), bias=1.0)
                ACT(sg[:, hs], sg[:, hs], AF.Exp, (), (b_sg,), scale=-1.0)
                yield
                TT("dve", sg[:, hs], sg[:, hs], cP[bank], ALU.mult, (), (b_cP[bank], b_sg))
                yield
            while qconv_done[0] < k - 1:
                yield
            for c in range(4):
                yield from projFg(cP[0], b_cP[0], c, 5 + c, l, hT, b_hT)
            CP("dve", DFw[:, 0:4, 3:131], v3(cP[0], 4), (), (b_cP[0], b_DFw))
            yield from projFg(cP[1], b_cP[1], 0, 9, l, hT, b_hT)
            yield from projFg(cP[1], b_cP[1], 1, 10, l, hT, b_hT)
            for kc in range(8):
                MM(cP[1][:, 256:264], hT[:, kc, :], Wt[:, l, kc, 128:136], (b_hT,), (b_cP[1],),
                   start=(kc == 0), stop=(kc == 7))
            CP("dve", DFw[:, 4:6, 3:131], v3(cP[1][:, 0:256], 2), (), (b_cP[1], b_DFw))
            CP("act", scG[:, BA:BA + 8], cP[1][:, 256:264], (), (b_cP[1], b_BA))
            yield

            if b == 0:
                MEMSET("pool", DFtail[:, s, l], 0.0, (b_DFt,))
            CP("pool", DFw[:, :, 0:3], DFtail[:, s, l], (b_DFt,), (b_DFw,))
            tmpc = gs[:, 3, 256:384]
            for ci in range(4):
                cw = PV_CONV + ci * 4
                TS("dve", cs[:, ci, :], DFw[:, ci, 3:131], pvec[:, l, cw + 3:cw + 4], None, ALU.mult, None,
                   (b_DFw, b_c), (b_cs,))
                for j in range(3):
                    STT(cs[:, ci, :], DFw[:, ci, j:j + 128], pvec[:, l, cw + j:cw + j + 1], cs[:, ci, :],
                        ALU.mult, ALU.add, (b_DFw, b_c), (b_cs,))
                if ci < 2:
                    cv = 4 + ci
                    cw = PV_CONV + cv * 4
                    TS("pool", cs[:, cv, :], DFw[:, cv, 3:131], pvec[:, l, cw + 3:cw + 4], 0.0, ALU.mult, ALU.add,
                       (b_DFw, b_c), (b_csv,))
                    for j in range(3):
                        TS("pool", tmpc, DFw[:, cv, j:j + 128], pvec[:, l, cw + j:cw + j + 1], 0.0, ALU.mult, ALU.add,
                           (b_DFw, b_c), (b_tmpv,))
                        TT("pool", cs[:, cv, :], cs[:, cv, :], tmpc, ALU.add, (b_tmpv,), (b_csv,))
                yield
            CP("pool", DFtail[:, s, l], DFw[:, :, 128:131], (b_DFw,), (b_DFt,))
            yield
            nkb = len(kbs)
            k0 = kbs[0]
            for g2 in range(2):
                for kb in kbs:
                    MM(cst_[:, kb * 512:(kb + 1) * 512], KF[:, s, l, ring[kb], :],
                       QFz[:, 4 * g2:4 * g2 + 4, :].rearrange("p c t -> p (c t)"), (b_KFs[s], b_QF), b_cst,
                       start=True, stop=False)
                    MM(cst_[:, kb * 512:(kb + 1) * 512].rearrange("p (c t) -> p c t", c=4), idb[:, :],
                       bc(swam[:, kb, :].unsqueeze(1), [128, 4, 128]), (b_c2,), b_cst, start=False, stop=True)
                ACT(pexp[:, k0 * 512:1024], cst_[:, k0 * 512:1024], AF.Exp, (), b_cst + (b_pexp,), scale=0.125)
                yield
                for qh in range(4):
                    for kb in kbs:
                        MM(cO[:, qh * 65:(qh + 1) * 65], pexp[:, kb * 512 + qh * 128:kb * 512 + (qh + 1) * 128],
                           Vc[:, s, l, ring[kb], g2, :], (b_pexp, b_V), (b_cO,), start=(kb == k0), stop=(kb == 1))
                po4 = v3(cO[:, 0:260], 4)
                TT("dve", scA[:, DEN:DEN + 4], po4[:, :, 64], lay[:, l, 4 * g2:4 * g2 + 4], ALU.add,
                   (b_c2,), (b_cO, b_sc))
                RECIP(scA[:, DEN:DEN + 4], scA[:, DEN:DEN + 4], (), (b_sc,))
                yield
                TT("dve", v3(mixed[:, 256 * g2:256 * g2 + 256], 4), po4[:, :, 0:64],
                   bc(scA[:, DEN:DEN + 4].unsqueeze(2), [128, 4, 64]), ALU.mult, (b_sc,), (b_cO, bmA))
                yield
            for h in range(4):
                for mb in range(2):
                    c0 = (h * 2 + mb) * 128
                    MM(cst_[:, c0:c0 + 128], mkT[:, l, s, h // 2, mb * 128:(mb + 1) * 128], MFz[:, h, :],
                       (b_mk, b_MF), b_cst)
                yield
            ACT(pexp, cst_, AF.Exp, (), b_cst + (b_pexp,), scale=0.125)
            yield
            for h in range(4):
                for mb in range(2):
                    c0 = (h * 2 + mb) * 128
                    MM(cO[:, h * 65:(h + 1) * 65], pexp[:, c0:c0 + 128], mv[:, l, s, mb, h, :],
                       (b_pexp, b_mk), (b_cO,), start=(mb == 0), stop=(mb == 1))
            po4 = v3(cO[:, 0:260], 4)
            CP("dve", scA[:, DEN:DEN + 4], po4[:, :, 64], (), (b_cO, b_sc))
            RECIP(scA[:, DEN:DEN + 4], scA[:, DEN:DEN + 4], (), (b_sc,))
            yield
            TT("dve", v3(mixed[:, 768:1024], 4), po4[:, :, 0:64],
               bc(scA[:, DEN:DEN + 4].unsqueeze(2), [128, 4, 64]), ALU.mult, (b_sc,), (b_cO, bmA))
            yield
            TT("pool", mixed[:, 0:512], mixed[:, 0:512], sg[:, 0:512], ALU.mult, (b_sg,), (bmA,))
            TT("pool", mixed[:, 768:1024], mixed[:, 768:1024], sg[:, 768:1024], ALU.mult, (b_sg,), (bmA,))
            yield

        def genR(s, b, l):
            slot = s
            bx = b_xs[slot]
            xt = xs[:, slot, :]
            sg, b_sg = sgs[s], b_sgs[s]
            mixed, bmA, bmG = mixeds[s], b_mixA[s], b_mixG[s]
            for kc in range(8):
                TR(cT[:, kc * 128:(kc + 1) * 128], mixed[:, kc * 128:(kc + 1) * 128], idb[:, :], (bmA, bmG), (b_cT,))
            CP("act", mixT.rearrange("p k t -> p (k t)"), cT[:, :], (), (b_cT, b_QF))
            yield
            for half in range(2):
                for kc in range(8):
                    MM(cP[half], mixT[:, kc, :], Wo[:, l, kc, half * 512:(half + 1) * 512],
                       (b_QF,), (b_cP[half],), start=(kc == 0), stop=(kc == 7))
                    yield
            for half in range(2):
                ACT(ytmp2, cP[half], AF.Square, (), (b_cP[half], b_sc, b_pexp),
                    accum=scA[:, SSY + half:SSY + half + 1])
            yield
            TT("dve", scA[:, SSY:SSY + 1], scA[:, SSY:SSY + 1], scA[:, SSY + 1:SSY + 2], ALU.add, (), (b_sc,))
            rsqrt_act(scA[:, SSY:SSY + 1], scA[:, SSY:SSY + 1], 1.0 / D, (), (b_sc,))
            yield
            for half in range(2):
                hs = slice(half * 512, (half + 1) * 512)
                STT(ytmp2, cP[half], scA[:, SSY:SSY + 1], rowb[:, l, RB_POST + half * 512:RB_POST + (half + 1) * 512],
                    ALU.mult, ALU.mult, (b_sc, b_c), (b_cP[half], b_pexp))
                TT("dve", xt[:, hs], xt[:, hs], ytmp2, ALU.add, (b_pexp,), (bx,))
                yield
            if l == DEPTH - 1:
                i = s * NBLK + b
                DMA(out_d[i * 128:(i + 1) * 128, :], xt, d_o[slot], (bx,), ())

        def genQ(s, b, l, k):
            mixed, bmG = mixeds[s], b_mixG[s]
            S32l, Sbfl, DFtl = S32[:, s, l], Sbf[:, s, l], DFtail[:, s, l]
            if b == 0:
                MEMSET("pool", S32l, 0.0, (b_S32,))
                MEMSET("pool", Sbfl, 0.0, (b_Sbf,))
            sqf = sq[:, :, :].rearrange("p c t -> p (c t)")
            csf = cs[:, 0:4, :].rearrange("p c t -> p (c t)")
            csv = cs[:, 4:6, :].rearrange("p c t -> p (c t)")
            tmpc = gs[:, 3, 256:384]
            gt_b = bc(scG[:, GTOK:GTOK + 4].unsqueeze(2), [128, 4, 128])
            beta_b = bc(scG[:, BETA:BETA + 4].unsqueeze(2), [128, 4, 128])
            egt_b = bc(scG[:, EGT:EGT + 4].unsqueeze(2), [128, 4, 128])
            gr4 = gB.rearrange("p (a b t) -> p a b t", a=2, b=2)

            def y1():
                ACT(scG[:, BETA:BETA + 4], scG[:, BA:BA + 4], AF.Exp, (b_BA,), (b_scG,), scale=-1.0)
                TT("dve", scG[:, GRAW:GRAW + 4], scG[:, BA + 4:BA + 8], rowb[:, l, RB_DTB:RB_DTB + 4], ALU.add,
                   (b_c, b_BA), (b_scG,))

            def y2():
                ACT(scG[:, GRAW:GRAW + 4], scG[:, GRAW:GRAW + 4], AF.Exp, (), (b_scG,))
                TS("dve", scG[:, BETA:BETA + 4], scG[:, BETA:BETA + 4], 1.0, None, ALU.add, None, (), (b_scG,))
                RECIP(scG[:, BETA:BETA + 4], scG[:, BETA:BETA + 4], (), (b_scG,))

            def y3():
                ACT(scG[:, GRAW:GRAW + 4], scG[:, GRAW:GRAW + 4], AF.Ln, (), (b_scG,), bias=1.0)
                TT("dve", scG[:, GRAW:GRAW + 4], scG[:, GRAW:GRAW + 4], lay[:, l, 8:12], ALU.mult, (b_c2,), (b_scG,))

            def y4():
                MM(gB[:, 0:4], utb, scG[:, GRAW:GRAW + 4], (b_scG, b_c), (b_gB,))
                CP("dve", scG[:, GTOK:GTOK + 4], gB[:, 0:4], (), (b_gB, b_scG))

            def y5():
                ACT(scG[:, EGT:EGT + 4], scG[:, GTOK:GTOK + 4], AF.Exp, (), (b_scG,))
                TT("dve", v3(Dt, 4), bc(ident.unsqueeze(1), [128, 4, 128]), gt_b, ALU.mult, (b_c, b_scG), (b_Dt,))

            def y6():
                MM(gB, onesf[:, :], Dt, (b_Dt, b_c2), (b_gB,))
                TT("dve", v3(Dd, 4), gt_b, v3(gB, 4), ALU.subtract, (b_scG,), (b_gB, b_Dd))
                ACT(eG[0:64, :, :], gr4[0:64, :, 0, :], AF.Exp, (), (b_gB, b_eG))
                ACT(eG[64:128, :, :], gr4[64:128, :, 1, :], AF.Exp, (), (b_gB, b_eG))

            def y7():
                TS("dve", Dt, Dd, 0.0, None, ALU.min, None, (b_Dd,), (b_Dt,))
                ACT(E1, Dt, AF.Exp, (b_Dt,), (b_E1,))

            def y8():
                TS("dve", Dt, Dd, -1.0, 0.0, ALU.mult, ALU.min, (b_Dd,), (b_Dt,))
                ACT(E2, Dt, AF.Exp, (b_Dt,), (b_E2,))

            def y9():
                TT("dve", v3(E1, 4), v3(E1, 4), beta_b, ALU.mult, (b_scG,), (b_E1,))

            def y10():
                TT("dve", v3(E1, 4), v3(E1, 4), bc(SLm.unsqueeze(1), [128, 4, 128]), ALU.mult, (b_c,), (b_E1,))

            def y11():
                TT("dve", v3(E2, 4), v3(E2, 4), bc(ULm.unsqueeze(1), [128, 4, 128]), ALU.mult, (b_c,), (b_E2,))

            def xconv(ci):
                def f():
                    cw = PV_CONV + ci * 4
                    TS("dve", cs[:, ci, :], DFw[:, ci, 3:131], pvec[:, l, cw + 3:cw + 4], None, ALU.mult, None,
                       (b_DFw, b_c), (b_cs,))
                    for j in range(3):
                        STT(cs[:, ci, :], DFw[:, ci, j:j + 128], pvec[:, l, cw + j:cw + j + 1], cs[:, ci, :],
                            ALU.mult, ALU.add, (b_DFw, b_c), (b_cs,))
                return f

            def vconv(ci):
                def f():
                    cw = PV_CONV + ci * 4
                    TS("pool", cs[:, ci, :], DFw[:, ci, 3:131], pvec[:, l, cw + 3:cw + 4], 0.0, ALU.mult, ALU.add,
                       (b_DFw, b_c), (b_csv,))
                    for j in range(3):
                        TS("pool", tmpc, DFw[:, ci, j:j + 128], pvec[:, l, cw + j:cw + j + 1], 0.0, ALU.mult, ALU.add,
                           (b_DFw, b_c), (b_tmpv,))
                        TT("pool", cs[:, ci, :], cs[:, ci, :], tmpc, ALU.add, (b_tmpv,), (b_csv,))
                return f

            def x5():
                ACT(sqf, csf, AF.Exp, (b_cs,), (b_sq,), scale=-1.0)

            def x6():
                ACT(sqf, sqf, AF.Ln, (), (b_sq,), bias=1.0)

            def x7():
                ACT(sqf, sqf, AF.Exp, (), (b_sq,), scale=-1.0)

            def x8():
                TT("dve", csf, csf, sqf, ALU.mult, (b_sq,), (b_cs,))

            def x9():
                TT("dve", sqb[:, :], csf, csf, ALU.mult, (b_cs,), (b_sqb,))
                MM(gA, bonesb[:, :], sqb[:, :], (b_sqb, b_c2), (b_gA,))

            def x10():
                ACT(sqf, gA, AF.Ln, (), (b_gA, b_sq), bias=EPS)

            def x11():
                ACT(sqf, sqf, AF.Exp, (), (b_sq,), scale=-0.5)

            def x12():
                TT("dve", qkn[:, 2:4, :], cs[:, 2:4, :], sq[:, 2:4, :], ALU.mult, (b_cs, b_sq), (b_qkn,))
                STT(qkn[:, 0:2, :], cs[:, 0:2, :], 0.125, sq[:, 0:2, :], ALU.mult, ALU.mult, (b_cs, b_sq), (b_qkn,))

            def x13():
                for h2 in range(2):
                    TS("dve", kTz[:, :, h2, :], qkn[:, 2:4, :], hm[:, h2:h2 + 1], None, ALU.mult, None,
                       (b_qkn, b_c), (b_kTz,))
                for pair in range(2):
                    TR(gT[:, pair * 128:(pair + 1) * 128], qkn[:, 2 + pair, :], idb[:, :], (b_qkn,), (b_gT,))
                CP("act", kvtok[:, 0, :, :].rearrange("p h d -> p (h d)"), gT[:, 0:256], (), (b_gT, b_kvtok))

            def x14():
                for h in range(4):
                    MM(gA[:, h * 128:(h + 1) * 128], kTz[:, h // 2, h % 2, :], qkn[:, 2 + h // 2, :],
                       (b_kTz, b_qkn), (b_gA,))
                for h in range(4):
                    MM(gC[:, h * 128:(h + 1) * 128], kTz[:, h // 2, h % 2, :], qkn[:, h // 2, :],
                       (b_kTz, b_qkn), (b_gC,))

            xs_ = [x5, x6, x7, x8, x9, x10, x11, x12, x13, x14]
            ys_ = [y1, y2, y3, y4, y5, y6, y7, y8, y9, y10, y11]
            vs_ = {}
            for n_ in range(max(len(xs_), len(ys_))):
                if n_ < len(ys_):
                    ys_[n_]()
                if n_ < len(xs_):
                    xs_[n_]()
                if n_ in vs_:
                    vs_[n_]()
                yield 7
            ACT(tmpv, csv, AF.Exp, (b_csv,), (b_tmpv,), scale=-1.0)
            ACT(tmpv, tmpv, AF.Ln, (), (b_tmpv,), bias=1.0)
            ACT(tmpv, tmpv, AF.Exp, (), (b_tmpv,), scale=-1.0)
            TT("dve", Aa[:, 0, :], gA, E1, ALU.mult, (b_E1,), (b_gA, b_A[0]))
            yield
            TT("pool", vT[:, :, :].rearrange("p c t -> p (c t)"), csv, tmpv, ALU.mult, (b_csv, b_tmpv), (b_vT,))
            qconv_done[0] = k
            for h in range(4):
                TR(gT[:, h * 128:(h + 1) * 128], Aa[:, 0, h * 128:(h + 1) * 128], idb[:, :], (b_A[0],), (b_gT,))
            for j2 in range(2):
                cidx = 64 * j2 + 63
                TT("pool", kdz[:, j2, :, :], kvtok[:, 0, :, :],
                   bc(v3(E2, 4)[:, :, cidx:cidx + 1], [128, 4, 64]), ALU.mult, (b_kvtok, b_E2), (b_kdz,))
            yield
            TT("dve", inT, gC, E2, ALU.mult, (b_E2,), (b_gC, b_inT))
            CP("act", Bb[:, 0, :], gT[:, 0:512], (), (b_gT, b_B[0]))
            yield
            TT("dve", v3(Pp[:, 0, :], 4), bc(ident.unsqueeze(1), [128, 4, 128]), v3(gT[:, 0:512], 4),
               ALU.subtract, (b_c,), (b_gT, b_P[0]))
            TT("pool", qg[:, :, :], qkn[:, 0:2, :], eG[:, :, :], ALU.mult, (b_qkn, b_eG), (b_qg,))
            for h2 in range(2):
                TS("pool", qgz[:, :, h2, :], qg[:, :, :], hm[:, h2:h2 + 1], 0.0, ALU.mult, ALU.add,
                   (b_qg, b_c), (b_qgz,))
            yield
            for j in range(1, 6):
                c_, n_ = (j - 1) % 2, j % 2
                for h in range(4):
                    hs = slice(h * 128, (h + 1) * 128)
                    MM(gA[:, hs], Bb[:, c_, hs], Aa[:, c_, hs], (b_A[c_], b_B[c_]), (b_gA,))
                if j < 5:
                    for h in range(4):
                        hs = slice(h * 128, (h + 1) * 128)
                        MM(gB[:, hs], Aa[:, c_, hs], Bb[:, c_, hs], (b_A[c_], b_B[c_]), (b_gB,))
                yield 3
                CP("act", Aa[:, n_, :], gA, (), (b_gA, b_A[n_]))
                if j < 5:
                    CP("dve", Bb[:, n_, :], gB, (), (b_gB, b_B[n_]))
                yield 0
                for h in range(4):
                    hs = slice(h * 128, (h + 1) * 128)
                    MM(gC[:, hs], Aa[:, n_, hs], Pp[:, c_, hs], (b_A[n_], b_P[c_]), (b_gC,))
                yield 2
                TT("dve", Pp[:, n_, :], Pp[:, c_, :], gC, ALU.add, (b_P[c_],), (b_gC, b_P[n_]))
                yield 0
            PT = Pp[:, 1, :]
            for pair in range(2):
                TR(gT[:, 256 + pair * 128:256 + (pair + 1) * 128], vT[:, pair, :], idb[:, :], (b_vT,), (b_gT,))
            CP("act", kvtok[:, 1, :, :].rearrange("p h d -> p (h d)"), gT[:, 256:512], (), (b_gT, b_kvtok))
            TT("dve", v3(Tb, 4), v3(PT, 4), beta_b, ALU.mult, (b_P[1], b_scG), (b_Tb,))
            yield
            TT("dve", v3(Wp, 4), v3(Tb, 4), egt_b, ALU.mult, (b_Tb, b_scG), (b_Wp,))
            for h in range(4):
                MM(gA[:, h * 64:(h + 1) * 64], Tb[:, h * 128:(h + 1) * 128], kvtok[:, 1, h, :],
                   (b_Tb, b_kvtok), (b_gA,))
            yield
            CP("act", uu, gA[:, 0:256], (), (b_gA, b_uo))
            yield
            for j2 in range(2):
                R_ = slice(64 * j2, 64 * j2 + 64)
                ts_ = slice(64 * j2, 64 * j2 + 64)
                for h in range(4):
                    MM(gB[R_, h * 64:(h + 1) * 64], kTz[:, h // 2, h % 2, ts_], Sbfl[:, h // 2, :],
                       (b_kTz, b_Sbf), (b_gB,))
                yield 3
                CP("act", kS[R_, :], gB[R_, 0:256], (), (b_gB, b_kS))
                yield 0
                for h in range(4):
                    MM(gA[R_, h * 64:(h + 1) * 64], Wp[:, h * 128 + 64 * j2:h * 128 + 64 * j2 + 64],
                       kS[:, h * 64:(h + 1) * 64], (b_Wp, b_kS), (b_gA,))
                yield 3
                TT("dve", vnew[R_, :], uu[R_, :], gA[R_, 0:256], ALU.subtract, (b_uo,), (b_gA, b_vnew))
                yield 0
                for h in range(4):
                    MM(gC[R_, h * 64:(h + 1) * 64], qgz[:, h // 2, h % 2, ts_], Sbfl[:, h // 2, :],
                       (b_qgz, b_Sbf), (b_gC,), start=True, stop=False)
                    MM(gC[R_, h * 64:(h + 1) * 64], inT[:, h * 128 + 64 * j2:h * 128 + 64 * j2 + 64],
                       vnew[:, h * 64:(h + 1) * 64], (b_inT, b_vnew), (b_gC,), start=False, stop=True)
                for h in range(4):
                    h2, pair = h % 2, h // 2
                    MM(gTf[64 * h2:64 * h2 + 64, pair * 64:(pair + 1) * 64], kdz[:, j2, h, :],
                       vnew[:, h * 64:(h + 1) * 64], (b_kdz, b_vnew), (b_gT,))
                yield 3
                for pair in range(2):
                    STT(Sbfl[:, pair, :], S32l[:, pair, :], eG[:, pair, 64 * j2 + 63:64 * j2 + 64],
                        gTf[:, pair * 64:(pair + 1) * 64], ALU.mult, ALU.add, (b_eG, b_S32), (b_gT, b_Sbf))
                yield
                for pair in range(2):
                    STT(S32l[:, pair, :], S32l[:, pair, :], eG[:, pair, 64 * j2 + 63:64 * j2 + 64],
                        gTf[:, pair * 64:(pair + 1) * 64], ALU.mult, ALU.add, (b_eG,), (b_gT, b_S32))
                yield
            ACT(osq, gC[:, 0:256], AF.Square, (), (b_gC, b_osq))
            yield
            K.op("dve", lambda e: e.tensor_reduce(scG[:, SS4:SS4 + 4], v3(osq, 4), AX.X, ALU.add),
                 (b_osq,), (b_scG,))
            rsqrt_act(scG[:, SS4:SS4 + 4], scG[:, SS4:SS4 + 4], 1.0 / 64, (), (b_scG,))
            yield
            TT("dve", v3(oo, 4), v3(gC[:, 0:256], 4), bc(scG[:, SS4:SS4 + 4].unsqueeze(2), [128, 4, 64]),
               ALU.mult, (b_scG,), (b_gC, b_uo))
            yield
            TT("pool", v3(mixed[:, 512:768], 4), v3(oo, 4),
               bc(rowb[:, l, RB_DNG:RB_DNG + 64].unsqueeze(1), [128, 4, 64]), ALU.mult,
               (b_uo, b_c), (bmG,))
            TT("pool", mixed[:, 512:768], mixed[:, 512:768], sgs[s][:, 512:768], ALU.mult, (b_sgs[s],), (bmG,))
            yield

        steps = [(s, b, l) for b in range(NBLK) for l in range(DEPTH) for s in range(NSEQ)]
        nst = len(steps)

        def drain(g):
            for _ in g:
                pass

        if NSEQ >= 2:
            drain(genP(*steps[0], 0))
            for k in range(nst):
                side = []
                if k >= 1:
                    side.append(genR(*steps[k - 1]))
                if k + 1 < nst:
                    side.append(genP(*steps[k + 1], k + 1))
                interleave(genQ(*steps[k], k), chain(*side), IL_RATIO, 1)
            drain(genR(*steps[nst - 1]))
        else:
            for k, st_ in enumerate(steps):
                qconv_done[0] = k
                drain(genP(*st_, k))
                drain(genQ(*st_, k))
                drain(genR(*st_))
        K.final_wait("sp", d_o + [d_dbg])
        K.emit()
    return nc, dbg_d


def _consts():
    c = np.zeros((128, NCONST), np.float32)
    p = np.arange(128)[:, None]
    f = np.arange(128)[None, :]
    same = (p // 64) == (f // 64)
    c[:, C_ID:C_ID + 128] = (p == f)
    c[:, C_SL:C_SL + 128] = same & ((p % 64) > (f % 64))
    c[:, C_UL:C_UL + 128] = same & ((f % 64) >= (p % 64))
    c[:, C_MPREV:C_MPREV + 128] = np.where(p > f, 0.0, -30000.0)
    c[:, C_MCUR:C_MCUR + 128] = np.where(p <= f, 0.0, -30000.0)
    c[:, C_BONES:C_BONES + 128] = same
    c[:, C_UTB:C_UTB + 128] = same & ((p % 64) <= (f % 64))
    c[:, C_HM + 0] = (np.arange(128) < 64)
    c[:, C_HM + 1] = (np.arange(128) >= 64)
    return c


def _prep_weights(w_in, w_out, w_mem_kv, pre_norm_g, mem_norm_g, conv_w, a_log, dt_bias, sinks, dn_norm_g,
                  post_norm_g):
    DEPTH = w_in.shape[0]
    qcols = []
    for c in range(4):
        qcols += list(range(O_SQ + c * 64, O_SQ + (c + 1) * 64)) + list(range(O_SQ + (4 + c) * 64, O_SQ + (5 + c) * 64))
    fcols = qcols + list(range(O_SK, O_SK + 128)) + list(range(O_DQ, O_DQ + 768)) + list(range(O_MQ, O_MQ + 256))
    tcols = list(range(O_SV, O_SV + 128)) + list(range(O_BETA, O_BETA + 8)) + list(range(O_GATE, O_GATE + 1024))
    assert len(fcols) == WFC and len(tcols) == WTC
    wall = np.empty((DEPTH, 128, 8, WALLC), np.float32)
    wkv = np.empty((DEPTH, 128, 8, 512), np.float32)
    for l in range(DEPTH):
        wi = w_in[l].reshape(8, 128, -1)
        wall[l, :, :, 0:WFC] = wi[:, :, fcols].transpose(1, 0, 2)
        wall[l, :, :, WFC:WFC + WTC] = wi[:, :, tcols].transpose(1, 0, 2)
        wall[l, :, :, WFC + WTC:] = w_out[l].reshape(8, 128, 1024).transpose(1, 0, 2)
        wkv[l] = w_mem_kv[l].reshape(8, 128, 512).transpose(1, 0, 2)
    pvec = np.zeros((128, DEPTH, NPV), np.float32)
    rowb = np.zeros((128, DEPTH, NRB), np.float32)
    for l in range(DEPTH):
        pvec[:, l, PV_GPRE:PV_GPRE + 8] = pre_norm_g[l].reshape(8, 128).T
        pvec[:, l, PV_GMEM:PV_GMEM + 8] = mem_norm_g[l].reshape(8, 128).T
        pvec[:, l, PV_CONV:PV_CONV + 24] = conv_w[l].reshape(4, 6, 128).transpose(2, 1, 0).reshape(128, 24)
        rowb[:, l, RB_POST:RB_POST + 1024] = post_norm_g[l][None, :]
        rowb[:, l, RB_DNG:RB_DNG + 64] = dn_norm_g[l][None, :]
        rowb[:, l, RB_SINK:RB_SINK + 8] = sinks[l][None, :]
        rowb[:, l, RB_ALOG:RB_ALOG + 4] = a_log[l][None, :]
        rowb[:, l, RB_DTB:RB_DTB + 4] = dt_bias[l][None, :]
    return wall, wkv, pvec, rowb


_NC_CACHE = {}


def run(x, mem, params, ncores, dbg_names=(), self_sync=True, trace=False):
    Bt, S, _ = x.shape
    NSEQ = Bt // ncores
    DEPTH = params["w_in"].shape[0]
    wall, wkv, pvec, rowb = _prep_weights(**params)
    consts = _consts()
    key = (NSEQ, S, DEPTH, tuple(dbg_names), self_sync)
    if key not in _NC_CACHE:
        _NC_CACHE[key] = build(NSEQ, S, DEPTH, dbg_names, self_sync)
    nc, dbg_d = _NC_CACHE[key]
    in_maps = []
    for c in range(ncores):
        in_maps.append({
            "x": np.ascontiguousarray(x[c * NSEQ:(c + 1) * NSEQ].reshape(NSEQ * S, D)),
            "mem": np.ascontiguousarray(mem[c * NSEQ:(c + 1) * NSEQ].reshape(NSEQ * 256, D)),
            "wall": wall, "wkv": wkv, "pvec": pvec, "rowb": rowb, "consts": consts,
        })
    res = run_bass_kernel_spmd(nc, in_maps, core_ids=list(range(ncores)), **({"trace": True} if trace else {}))
    out = np.concatenate([r["out"].reshape(NSEQ, S, D) for r in res.results], axis=0)
    return out, res


def kernel(x, mem, pre_norm_g, w_in, conv_w, a_log, dt_bias, sinks, dn_norm_g, mem_norm_g, w_mem_kv, w_out,
           post_norm_g):
    f = lambda a: np.asarray(a, dtype=np.float32)
    params = dict(w_in=f(w_in), w_out=f(w_out), w_mem_kv=f(w_mem_kv), pre_norm_g=f(pre_norm_g),
                  mem_norm_g=f(mem_norm_g), conv_w=f(conv_w), a_log=f(a_log), dt_bias=f(dt_bias),
                  sinks=f(sinks), dn_norm_g=f(dn_norm_g), post_norm_g=f(post_norm_g))
    out, _ = run(f(x), f(mem), params, NCORES)
    return out.astype(np.float32)
```

```python
import contextlib
import numpy as np
import concourse.bass as bass
import concourse.mybir as mybir
from concourse.bass_utils import run_bass_kernel_spmd

F32 = mybir.dt.float32
BF16 = mybir.dt.bfloat16
AF = mybir.ActivationFunctionType
ALU = mybir.AluOpType
AX = mybir.AxisListType

D = 1024
NCORES = 8
EPS = 1e-6
EPOCH = 12000
IL_RATIO = 2
O_SQ, O_SK, O_SV, O_DQ, O_DK, O_DV, O_BETA, O_ALPHA, O_MQ, O_GATE = 0, 512, 640, 768, 1024, 1280, 1536, 1540, 1544, 1800
NF = 13
WFC = NF * 128
WTC = 128 + 8 + 1024
WALLC = WFC + WTC + 1024
PV_GPRE, PV_GMEM, PV_CONV = 0, 8, 16
NPV = 40
RB_POST, RB_DNG, RB_SINK, RB_ALOG, RB_DTB = 0, 1024, 1088, 1096, 1100
NRB = 1104
C_ID, C_SL, C_UL, C_BONES, C_UTB, C_HM, C_MPREV, C_MCUR = 0, 128, 256, 384, 512, 640, 644, 772
NCST = 644
NCONST = 900


class Sem:
    def __init__(self, h, owner):
        self.h = h
        self.owner = owner
        self.count = 0


class Buf:
    __slots__ = ("name", "w", "r")

    def __init__(self, name):
        self.name = name
        self.w = None
        self.r = {}


class Eng:
    def __init__(self, name, self_sync):
        self.name = name
        self.self_sync = self_sync
        self.sem = None
        self.known = {}
        self.prog = []


class KB:
    def __init__(self, nc, stack, self_sync=True):
        self.nc = nc
        self.stack = stack
        self.nsem = 0
        self.engs = {n: Eng(n, ss) for n, ss in
                     (("pe", False), ("dve", self_sync), ("act", self_sync), ("pool", self_sync), ("sp", False))}
        for e in self.engs.values():
            e.sem = self.new_sem(e.name)

    def new_sem(self, owner):
        h = self.stack.enter_context(self.nc.semaphore(f"s{self.nsem}_{owner}"))
        self.nsem += 1
        return Sem(h, owner)

    def _waits(self, E, reads, writes):
        need = {}

        def req(sv):
            if sv is None:
                return
            s, v = sv
            if s.owner == E.name and not E.self_sync:
                return
            if need.get(s, 0) < v:
                need[s] = v
        for b in reads:
            req(b.w)
        for b in writes:
            req(b.w)
            for s, v in b.r.items():
                req((s, v))
        wl = []
        for s, v in need.items():
            if E.known.get(s, 0) < v:
                E.known[s] = v
                wl.append((s.h, v))
        return wl

    def op(self, e, fn, reads=(), writes=()):
        E = self.engs[e]
        wl = self._waits(E, reads, writes)
        if E.sem.count >= EPOCH:
            E.sem = self.new_sem(E.name)
        sem = E.sem
        sem.count += 1
        val = sem.count
        E.prog.append((wl, fn, sem.h, 1))
        for b in reads:
            b.r[sem] = val
        for b in writes:
            b.w = (sem, val)
            b.r = {}

    def dma(self, e, fn, dsem, reads=(), writes=()):
        E = self.engs[e]
        wl = self._waits(E, reads, writes)
        dsem.count += 16
        val = dsem.count
        E.prog.append((wl, fn, dsem.h, 16))
        for b in reads:
            b.r[dsem] = val
        for b in writes:
            b.w = (dsem, val)
            b.r = {}

    def barrier(self):
        for E in self.engs.values():
            wl = []
            for O in self.engs.values():
                if O is E:
                    continue
                if O.sem.count > 0 and E.known.get(O.sem, 0) < O.sem.count:
                    E.known[O.sem] = O.sem.count
                    wl.append((O.sem.h, O.sem.count))
            if wl:
                E.prog.append((wl, None, None, 0))

    def final_wait(self, e, sems):
        E = self.engs[e]
        wl = [(s.h, s.count) for s in sems if s.count > 0]
        E.prog.append((wl, None, None, 0))

    def emit(self):
        nc = self.nc
        progs = {n: E.prog for n, E in self.engs.items()}

        def run(eng, prog):
            for wl, fn, semh, inc in prog:
                for h, v in wl:
                    eng.wait_ge(h, v)
                if fn is not None:
                    fn(eng).then_inc(semh, inc)

        with nc.Block() as block:
            @block.tensor
            def _(eng):
                run(eng, progs["pe"])

            @block.vector
            def _(eng):
                run(eng, progs["dve"])

            @block.scalar
            def _(eng):
                run(eng, progs["act"])

            @block.gpsimd
            def _(eng):
                run(eng, progs["pool"])

            @block.sync
            def _(eng):
                run(eng, progs["sp"])


def build(NSEQ, S, DEPTH, dbg_names=(), self_sync=True):
    NBLK = S // 128
    nc = bass.Bass("TRN2", target_bir_lowering=False)
    dt = nc.dram_tensor
    x_d = dt("x", [NSEQ * S, D], F32, kind="ExternalInput").ap()
    mem_d = dt("mem", [NSEQ * 256, D], F32, kind="ExternalInput").ap()
    wall_d = dt("wall", [DEPTH, 128, 8, WALLC], F32, kind="ExternalInput").ap()
    wkv_d = dt("wkv", [DEPTH, 128, 8, 512], F32, kind="ExternalInput").ap()
    pvec_d = dt("pvec", [128, DEPTH, NPV], F32, kind="ExternalInput").ap()
    rowb_d = dt("rowb", [128, DEPTH, NRB], F32, kind="ExternalInput").ap()
    const_d = dt("consts", [128, NCONST], F32, kind="ExternalInput").ap()
    out_d = dt("out", [NSEQ * S, D], F32, kind="ExternalOutput").ap()
    dbg_d = {}

    with contextlib.ExitStack() as st:
        K = KB(nc, st, self_sync)

        def sb(name, shape, dtype):
            return st.enter_context(nc.sbuf_tensor(name, shape, dtype))

        Wf = sb("Wf", [128, DEPTH, 8, WFC], BF16)
        Wt = sb("Wt", [128, DEPTH, 8, WTC], BF16)
        Wo = sb("Wo", [128, DEPTH, 8, 1024], BF16)
        pvec = sb("pvec_s", [128, DEPTH, NPV], F32)
        rowb = sb("rowb_s", [128, DEPTH, NRB], F32)
        cst = sb("cst", [128, NCST], F32)
        onesf = sb("onesf", [128, 128], F32)
        idb = sb("idb", [128, 128], BF16)
        swam = sb("swam", [128, 2, 128], BF16)
        lay = sb("lay", [128, DEPTH, 16], F32)
        scratch = sb("scratch", [128, 4096], BF16)
        wkv = scratch[:, :].rearrange("p (k c) -> p k c", k=8)
        pexp = scratch[:, 0:1024]
        mixed1 = sb("mixed1", [128, 1024], BF16)
        sgs = [scratch[:, 1024:2048], scratch[:, 2048:3072]]
        mixeds = [scratch[:, 3072:4096], mixed1[:, :]]
        KF = sb("KF", [128, NSEQ, DEPTH, 2, 128], BF16)
        Vc = sb("Vc", [128, NSEQ, DEPTH, 2, 2, 65], BF16)
        DFtail = sb("DFtail", [128, NSEQ, DEPTH, 6, 3], F32)
        DFw = sb("DFw", [128, 6, 131], F32)
        S32 = sb("S32", [128, NSEQ, DEPTH, 2, 64], F32)
        Sbf = sb("Sbf", [128, NSEQ, DEPTH, 2, 64], BF16)
        mkT = sb("mkT", [128, DEPTH, NSEQ, 2, 256], BF16)
        mv = sb("mv", [128, DEPTH, NSEQ, 2, 4, 65], BF16)
        xs = sb("xs", [128, 2, D], F32)
        tT = sb("tT", [128, 2, 8, 128], BF16)
        QM = sb("QM", [128, 12, 128], BF16)
        QFz = QM[:, 0:8, :]
        MFz = QM[:, 8:12, :]
        mixT = QM[:, 0:8, :]
        cs = sb("cs", [128, 6, 128], F32)
        sq = sb("sq", [128, 4, 128], F32)
        qkn = sb("qkn", [128, 4, 128], BF16)
        vT = sb("vT", [128, 2, 128], BF16)
        kTz = sb("kTz", [128, 2, 2, 128], BF16)
        qg = sb("qg", [128, 2, 128], BF16)
        qgz = sb("qgz", [128, 2, 2, 128], BF16)
        eG = sb("eG", [128, 2, 128], F32)
        kvtok = sb("kvtok", [128, 2, 4, 64], BF16)
        kdz = sb("kdz", [128, 2, 4, 64], BF16)
        sc = sb("sc", [128, 64], F32)
        gs = sb("gs", [128, 4, 512], F32)
        Aa = sb("Aa", [128, 2, 512], BF16)
        Bb = sb("Bb", [128, 2, 512], BF16)
        Pp = sb("Pp", [128, 2, 512], BF16)
        gb = sb("gb", [128, 3, 512], BF16)
        ksv = sb("ksv", [128, 2, 256], BF16)
        psum = st.enter_context(nc.psum_tensor("psum", [128, 4096], F32))

        hTs = [tT[:, 0], tT[:, 1]]
        hbf0 = gs[:, 0, :].bitcast(BF16)
        Dd = gs[:, 0, :]
        Dt = gs[:, 1, :]
        uu = gs[:, 2, 0:256]
        oo = gs[:, 2, 256:512]
        osq = gs[:, 3, 0:256]
        ytmp = gs[:, 0:2, :].rearrange("p a b -> p (a b)")
        E1 = Pp[:, 0, :]
        E2 = Pp[:, 1, :]
        Tb, Wp, inT = (gb[:, i, :] for i in range(3))
        kS = ksv[:, 0, :]
        vnew = ksv[:, 1, :]

        def pbank(i, n=1):
            return psum[:, i * 512:(i + n) * 512]
        pj = [pbank(0), pbank(1)]
        ptrf = pbank(2)
        ptr = pbank(2).bitcast(BF16)
        pst = pbank(3, 2)
        po = pbank(5)
        pgA = pbank(6)
        pgB = pbank(7)

        B = {}

        def bf(n):
            if n not in B:
                B[n] = Buf(n)
            return B[n]
        b_pj = [bf("pj0"), bf("pj1")]
        b_ptr, b_po, b_pgA, b_pgB = bf("ptr"), bf("po"), bf("pgA"), bf("pgB")
        b_pst = (bf("ps3"), bf("ps4"))
        b_xs = [bf("xs0"), bf("xs1")]
        b_Dd, b_Dt, b_uo, b_osq = bf("Dd"), bf("Dt"), bf("uo"), bf("osq")
        b_yt = (b_Dd, b_Dt)
        b_E1, b_E2 = bf("P0"), bf("P1")
        b_Tb, b_Wp, b_inT = bf("Tb"), bf("Wp"), bf("inT")
        b_kS, b_vnew = bf("kS"), bf("vnew")
        b_A = [bf("A0"), bf("A1")]
        b_B = [bf("B0"), bf("B1")]
        b_P = [bf("P0"), bf("P1")]

        d_x = [K.new_sem("dx0"), K.new_sem("dx1")]
        d_o = [K.new_sem("do0"), K.new_sem("do1")]
        d_c = K.new_sem("dc")
        d_dbg = K.new_sem("ddbg")

        def MM(out, lhsT, rhs, reads, writes, start=True, stop=True):
            K.op("pe", lambda e: e.matmul(out, lhsT, rhs, start=start, stop=stop), reads, writes)

        def TR(out, in_, ident, reads, writes):
            K.op("pe", lambda e: e.transpose(out, in_, ident), reads, writes)

        def ACT(out, in_, func, reads, writes, bias=None, scale=None, accum=None):
            kw = {}
            if bias is not None:
                kw["bias"] = bias
            if scale is not None:
                kw["scale"] = scale
            if accum is not None:
                kw["accum_out"] = accum
            K.op("act", lambda e: e.activation(out, in_, func, **kw), reads, writes)

        def TT(eng, out, in0, in1, op, reads, writes):
            K.op(eng, lambda e: e.tensor_tensor(out, in0, in1, op), reads, writes)

        def TS(eng, out, in0, s1, s2, op0, op1, reads, writes):
            if op1 is None:
                K.op(eng, lambda e: e.tensor_scalar(out, in0, s1, None, op0), reads, writes)
            else:
                K.op(eng, lambda e: e.tensor_scalar(out, in0, s1, s2, op0, op1), reads, writes)

        def STT(out, in0, scalar, in1, op0, op1, reads, writes):
            K.op("dve", lambda e: e.scalar_tensor_tensor(out, in0, scalar, in1, op0, op1), reads, writes)

        def CP(eng, out, in_, reads, writes):
            if eng == "act":
                K.op("act", lambda e: e.copy(out, in_), reads, writes)
            else:
                K.op(eng, lambda e: e.tensor_copy(out, in_), reads, writes)

        def RECIP(out, in_, reads, writes):
            K.op("dve", lambda e: e.reciprocal(out, in_), reads, writes)

        def MEMSET(eng, ap, val, writes):
            K.op(eng, lambda e: e.memset(ap, val), (), writes)

        def DMA(out, in_, dsem, reads, writes):
            K.dma("sp", lambda e: e.dma_start(out=out, in_=in_), dsem, reads, writes)

        def dbg(name, ap, shape, reads, dtype=F32):
            if name not in dbg_names or name in dbg_d:
                return
            t = dt("dbg_" + name, list(shape), dtype, kind="ExternalOutput").ap()
            dbg_d[name] = t
            DMA(t, ap, d_dbg, reads, ())

        def bc(ap, shape):
            return ap.broadcast_to(list(shape))

        def v3(ap, a):
            return ap.rearrange("p (a b) -> p a b", a=a)

        b_c = bf("consts")
        DMA(cst[:, :], const_d[:, 0:NCST], d_c, (), (b_c,))
        DMA(xs[:, 0, 0:256], const_d[:, C_MPREV:C_MPREV + 256], d_c, (), (b_c,))
        DMA(pvec[:, :, :], pvec_d, d_c, (), (b_c,))
        DMA(rowb[:, :, :], rowb_d, d_c, (), (b_c,))
        b_c.w = (d_c, d_c.count)
        b_c2 = bf("consts2")
        CP("dve", idb[:, :], cst[:, C_ID:C_ID + 128], (b_c,), (b_c2,))
        CP("dve", swam[:, :, :], v3(xs[:, 0, 0:256], 2), (b_c,), (b_c2, b_xs[0]))
        MEMSET("pool", onesf[:, :], 1.0, (b_c2,))
        for l in range(DEPTH):
            ACT(lay[:, l, 0:8], rowb[:, l, RB_SINK:RB_SINK + 8], AF.Exp, (b_c,), (b_c2,))
            ACT(lay[:, l, 8:12], rowb[:, l, RB_ALOG:RB_ALOG + 4], AF.Exp, (b_c,), (b_c2,))
            TS("dve", lay[:, l, 8:12], lay[:, l, 8:12], -1.0, None, ALU.mult, None, (b_c2,), (b_c2,))
        b_st = bf("state")
        MEMSET("pool", Vc[:, :, :, :, :, 64:65], 1.0, (b_st,))
        MEMSET("pool", mv[:, :, :, :, :, 64:65], 1.0, (b_st,))
        MEMSET("pool", ksv[:, :, :], 0.0, (b_kS, b_vnew))
        ident = cst[:, C_ID:C_ID + 128]
        SLm = cst[:, C_SL:C_SL + 128]
        ULm = cst[:, C_UL:C_UL + 128]
        bones = cst[:, C_BONES:C_BONES + 128]
        utb = cst[:, C_UTB:C_UTB + 128]
        hm = cst[:, C_HM:C_HM + 2]

        b_W = bf("W")
        b_wkv = bf("wkv")
        b_hbf, b_sc, b_tT = bf("mixed"), bf("sc"), bf("tT")
        b_mk = bf("mk")
        npiece = 0
        for l in range(DEPTH):
            for kc in range(8):
                pieces = [(Wf, 0, 0, 1024, True), (Wf, 1024, 1024, WFC - 1024, True),
                          (Wt, 0, WFC, 1024, True), (Wt, 1024, WFC + 1024, WTC - 1024, True),
                          (Wo, 0, WFC + WTC, 1024, False)]
                for (dst, dcol, scol, n, gain) in pieces:
                    slot = npiece % 2
                    npiece += 1
                    DMA(xs[:, slot, 0:n], wall_d[l, :, kc, scol:scol + n], d_x[slot], (), (b_xs[slot],))
                    eng = "dve" if slot == 0 else "pool"
                    if gain:
                        TS(eng, dst[:, l, kc, dcol:dcol + n], xs[:, slot, 0:n],
                           pvec[:, l, PV_GPRE + kc:PV_GPRE + kc + 1], 0.0, ALU.mult, ALU.add,
                           (b_xs[slot], b_c), (b_W,))
                    else:
                        CP(eng, dst[:, l, kc, dcol:dcol + n], xs[:, slot, 0:n], (b_xs[slot],), (b_W,))
            for k2 in range(4):
                slot = npiece % 2
                npiece += 1
                DMA(v3(xs[:, slot, :], 2), wkv_d[l, :, 2 * k2:2 * k2 + 2, :], d_x[slot], (), (b_xs[slot],))
                for kk in range(2):
                    kc = 2 * k2 + kk
                    TS("dve", wkv[:, kc, :], xs[:, slot, kk * 512:(kk + 1) * 512],
                       pvec[:, l, PV_GMEM + kc:PV_GMEM + kc + 1], 0.0, ALU.mult, ALU.add,
                       (b_xs[slot], b_c), (b_wkv,))
            for s in range(NSEQ):
                for mb in range(2):
                    slot = npiece % 2
                    npiece += 1
                    r0 = s * 256 + mb * 128
                    DMA(xs[:, slot, :], mem_d[r0:r0 + 128, :], d_x[slot], (), (b_xs[slot],))
                    ACT(hbf0, xs[:, slot, :], AF.Square, (b_xs[slot],), (b_hbf, b_sc), accum=sc[:, 0:1])
                    ACT(sc[:, 1:2], sc[:, 0:1], AF.Sqrt, (b_sc,), (b_sc,), bias=EPS, scale=1.0 / D)
                    RECIP(sc[:, 2:3], sc[:, 1:2], (b_sc,), (b_sc,))
                    TS("dve", hbf0, xs[:, slot, :], sc[:, 2:3], None, ALU.mult, None,
                       (b_xs[slot], b_sc), (b_hbf,))
                    for kc in range(8):
                        TR(ptr[:, kc * 128:(kc + 1) * 128], hbf0[:, kc * 128:(kc + 1) * 128], idb[:, :],
                           (b_hbf, b_c2), (b_ptr,))
                    CP("act", tT[:, mb].rearrange("p k t -> p (k t)"), ptr[:, :], (), (b_ptr, b_tT))
                for pair in range(2):
                    for kc in range(8):
                        MM(pj[0][:, 0:256], wkv[:, kc, pair * 128:(pair + 1) * 128], tT[:, :, kc, :],
                           (b_wkv, b_tT), (b_pj[0],), start=(kc == 0), stop=(kc == 7))
                    CP("act", mkT[:, l, s, pair, :], pj[0][:, 0:256], (), (b_pj[0], b_mk))
                for mb in range(2):
                    for kc in range(8):
                        MM(pj[1][:, 0:256], tT[:, mb, kc, :], wkv[:, kc, 256:512],
                           (b_wkv, b_tT), (b_pj[1],), start=(kc == 0), stop=(kc == 7))
                    CP("dve", mv[:, l, s, mb, :, 0:64], v3(pj[1][:, 0:256], 4),
                       (b_st,), (b_pj[1], b_mk))
                dbg(f"mkT{l}", mkT[:, l, s], [128, 2, 256], (b_mk,), BF16)
                dbg(f"mv{l}", mv[:, l, s], [128, 2, 4, 65], (b_mk,), BF16)
        K.barrier()

        cP = [pbank(0), pbank(1)]
        cst_ = pbank(0, 2)
        cO = pbank(2)
        cT = pbank(3).bitcast(BF16)
        gA, gB, gC = pbank(4), pbank(5), pbank(6)
        gTf = pbank(7)
        gT = pbank(7).bitcast(BF16)
        b_cP = [bf("pj0"), bf("pj1")]
        b_cst = (b_cP[0], b_cP[1])
        b_cO, b_cT = bf("ptr"), bf("ps3")
        b_gA, b_gB, b_gC, b_gT = bf("ps4"), bf("po"), bf("pgA"), bf("pgB")
        b_mixA = [bf("mixA0"), bf("mixA1")]
        b_mixG = [bf("mixG0"), bf("mixG1")]
        b_sgs = [bf("sg0"), bf("sg1")]
        b_hTs = [bf("hT0"), bf("hT1")]
        b_QF, b_MF, b_V = bf("QF"), bf("MF"), bf("V")
        b_KFs = [bf("KF0"), bf("KF1")]
        b_DFw, b_DFt, b_cs, b_sq = bf("DFw"), bf("DFt"), bf("cs"), bf("sq")
        b_qkn, b_vT, b_kTz, b_eG, b_qg, b_qgz = (bf(n) for n in ("qkn", "vT", "kTz", "eG", "qg", "qgz"))
        b_kvtok, b_kdz, b_pexp = bf("kvtok"), bf("kdz"), bf("pexp")
        b_S32, b_Sbf = bf("S32"), bf("Sbf")
        b_scG, b_tmpv, b_csv = bf("scG"), bf("tmpv"), bf("csv")
        tmpv = gs[:, 3, 256:512]
        ytmp2 = pexp.bitcast(F32)
        SS, RT, RSTD, DEN, SSY = 0, 1, 2, 4, 8
        BA, BETA, GRAW, GTOK, EGT, BGE, SS4 = 0, 8, 12, 16, 20, 24, 28
        scA = sc[:, 0:16]
        scG = sc[:, 16:64]

        def rsqrt_act(out, in_, scale, reads, writes):
            ACT(out, in_, AF.Ln, reads, writes, bias=EPS, scale=scale)
            ACT(out, out, AF.Exp, (), writes, scale=-0.5)

        def chain(*gens):
            for g in gens:
                yield from g

        def interleave(g1, g2, r1, r2):
            d1 = d2 = False
            while not (d1 and d2):
                for _ in range(r1):
                    if not d1:
                        try:
                            next(g1)
                        except StopIteration:
                            d1 = True
                for _ in range(r2):
                    if not d2:
                        try:
                            next(g2)
                        except StopIteration:
                            d2 = True

        def projF(bankap, bbank, pos, fc, l, hT, b_hT):
            for kc in range(8):
                MM(bankap[:, pos * 128:(pos + 1) * 128], Wf[:, l, kc, fc * 128:(fc + 1) * 128], hT[:, kc, :],
                   (b_hT,), (bbank,), start=(kc == 0), stop=(kc == 7))

        def genP(s, b, l):
            slot = s
            bx = b_xs[slot]
            xt = xs[:, slot, :]
            hT, b_hT = hTs[s], b_hTs[s]
            sg, b_sg = sgs[s], b_sgs[s]
            mixed, bmA, bmG = mixeds[s], b_mixA[s], b_mixG[s]
            hbf = mixed
            cur, prv = b % 2, 1 - (b % 2)
            kbs = [1] if b == 0 else [0, 1]
            ring = {0: prv, 1: cur}
            if l == 0:
                i = s * NBLK + b
                DMA(xt, x_d[i * 128:(i + 1) * 128, :], d_x[slot], (), (bx,))
            ACT(hbf, xt, AF.Square, (bx,), (bmA, bmG, b_sc), accum=scA[:, SS:SS + 1])
            rsqrt_act(scA[:, RSTD:RSTD + 1], scA[:, SS:SS + 1], 1.0 / D, (), (b_sc,))
            yield
            TS("dve", hbf, xt, scA[:, RSTD:RSTD + 1], None, ALU.mult, None, (bx, b_sc), (bmA, bmG))
            yield
            for kc in range(8):
                TR(cT[:, kc * 128:(kc + 1) * 128], hbf[:, kc * 128:(kc + 1) * 128], idb[:, :], (bmA, bmG), (b_cT,))
            CP("act", hT.rearrange("p k t -> p (k t)"), cT[:, :], (), (b_cT, b_hT))
            yield
            for c in range(4):
                projF(cP[0], b_cP[0], c, c, l, hT, b_hT)
                yield
            TT("dve", QFz.rearrange("p (g c) t -> p g c t", g=2), bc(v3(cP[0], 4).unsqueeze(1), [128, 2, 4, 128]),
               bc(hm.unsqueeze(2).unsqueeze(3), [128, 2, 4, 128]), ALU.mult, (b_c,), (b_cP[0], b_QF))
            projF(cP[1], b_cP[1], 0, 4, l, hT, b_hT)
            yield
            projF(cP[1], b_cP[1], 1, 11, l, hT, b_hT)
            yield
            projF(cP[1], b_cP[1], 2, 12, l, hT, b_hT)
            CP("act", KF[:, s, l, cur, :], cP[1][:, 0:128], (), (b_cP[1], b_KFs[s]))
            TT("dve", MFz.rearrange("p (a b) t -> p a b t", a=2),
               bc(v3(cP[1][:, 128:384], 2).unsqueeze(2), [128, 2, 2, 128]),
               bc(hm.unsqueeze(1).unsqueeze(3), [128, 2, 2, 128]), ALU.mult, (b_c,), (b_cP[1], b_MF))
            yield
            for kc in range(8):
                MM(cP[0][:, 0:128], hT[:, kc, :], Wt[:, l, kc, 0:128], (b_hT,), (b_cP[0],),
                   start=(kc == 0), stop=(kc == 7))
            CP("act", Vc[:, s, l, cur, :, 0:64], v3(cP[0][:, 0:128], 2), (b_st,), (b_cP[0], b_V))
            yield
            for half in range(2):
                bank = 1 - half
                hs = slice(half * 512, (half + 1) * 512)
                for kc in range(8):
                    MM(cP[bank], hT[:, kc, :], Wt[:, l, kc, 136 + half * 512:136 + (half + 1) * 512],
                       (b_hT,), (b_cP[bank],), start=(kc == 0), stop=(kc == 7))
                ACT(sg[:, hs], cP[bank], AF.Exp, (), (b_cP[bank], b_sg), scale=-1.0)
                yield
                ACT(sg[:, hs], sg[:, hs], AF.Ln, (), (b_sg,), bias=1.0)
                ACT(sg[:, hs], sg[:, hs], AF.Exp, (), (b_sg,), scale=-1.0)
                yield
                TT("dve", sg[:, hs], sg[:, hs], cP[bank], ALU.mult, (), (b_cP[bank], b_sg))
                yield
            nkb = len(kbs)
            k0 = kbs[0]
            for g2 in range(2):
                for kb in kbs:
                    MM(cst_[:, kb * 512:(kb + 1) * 512], KF[:, s, l, ring[kb], :],
                       QFz[:, 4 * g2:4 * g2 + 4, :].rearrange("p c t -> p (c t)"), (b_KFs[s], b_QF), b_cst)
                ACT(pexp[:, k0 * 512:1024], cst_[:, k0 * 512:1024], AF.Exp, (), b_cst + (b_pexp,), scale=0.125)
                yield
                pv4 = pexp[:, k0 * 512:1024].rearrange("p (k c t) -> p k c t", k=nkb, c=4)
                TT("pool", pv4, pv4, bc(swam[:, k0:2, :].unsqueeze(2), [128, nkb, 4, 128]), ALU.mult,
                   (b_c2,), (b_pexp,))
                yield
                for qh in range(4):
                    for kb in kbs:
                        MM(cO[:, qh * 65:(qh + 1) * 65], pexp[:, kb * 512 + qh * 128:kb * 512 + (qh + 1) * 128],
                           Vc[:, s, l, ring[kb], g2, :], (b_pexp, b_V), (b_cO,), start=(kb == k0), stop=(kb == 1))
                po4 = v3(cO[:, 0:260], 4)
                TT("dve", scA[:, DEN:DEN + 4], po4[:, :, 64], lay[:, l, 4 * g2:4 * g2 + 4], ALU.add,
                   (b_c2,), (b_cO, b_sc))
                RECIP(scA[:, DEN:DEN + 4], scA[:, DEN:DEN + 4], (), (b_sc,))
                yield
                TT("dve", v3(mixed[:, 256 * g2:256 * g2 + 256], 4), po4[:, :, 0:64],
                   bc(scA[:, DEN:DEN + 4].unsqueeze(2), [128, 4, 64]), ALU.mult, (b_sc,), (b_cO, bmA))
                yield
            for h in range(4):
                for mb in range(2):
                    c0 = (h * 2 + mb) * 128
                    MM(cst_[:, c0:c0 + 128], mkT[:, l, s, h // 2, mb * 128:(mb + 1) * 128], MFz[:, h, :],
                       (b_mk, b_MF), b_cst)
                yield
            ACT(pexp, cst_, AF.Exp, (), b_cst + (b_pexp,), scale=0.125)
            yield
            for h in range(4):
                for mb in range(2):
                    c0 = (h * 2 + mb) * 128
                    MM(cO[:, h * 65:(h + 1) * 65], pexp[:, c0:c0 + 128], mv[:, l, s, mb, h, :],
                       (b_pexp, b_mk), (b_cO,), start=(mb == 0), stop=(mb == 1))
            po4 = v3(cO[:, 0:260], 4)
            CP("dve", scA[:, DEN:DEN + 4], po4[:, :, 64], (), (b_cO, b_sc))
            RECIP(scA[:, DEN:DEN + 4], scA[:, DEN:DEN + 4], (), (b_sc,))
            yield
            TT("dve", v3(mixed[:, 768:1024], 4), po4[:, :, 0:64],
               bc(scA[:, DEN:DEN + 4].unsqueeze(2), [128, 4, 64]), ALU.mult, (b_sc,), (b_cO, bmA))
            yield

        def genR(s, b, l):
            slot = s
            bx = b_xs[slot]
            xt = xs[:, slot, :]
            sg, b_sg = sgs[s], b_sgs[s]
            mixed, bmA, bmG = mixeds[s], b_mixA[s], b_mixG[s]
            TT("pool", mixed, mixed, sg, ALU.mult, (b_sg,), (bmA, bmG))
            yield
            for kc in range(8):
                TR(cT[:, kc * 128:(kc + 1) * 128], mixed[:, kc * 128:(kc + 1) * 128], idb[:, :], (bmA, bmG), (b_cT,))
            CP("act", mixT.rearrange("p k t -> p (k t)"), cT[:, :], (), (b_cT, b_QF))
            yield
            for half in range(2):
                for kc in range(8):
                    MM(cP[half], mixT[:, kc, :], Wo[:, l, kc, half * 512:(half + 1) * 512],
                       (b_QF,), (b_cP[half],), start=(kc == 0), stop=(kc == 7))
                yield
            for half in range(2):
                ACT(ytmp2, cP[half], AF.Square, (), (b_cP[half], b_sc, b_pexp),
                    accum=scA[:, SSY + half:SSY + half + 1])
            yield
            TT("dve", scA[:, SSY:SSY + 1], scA[:, SSY:SSY + 1], scA[:, SSY + 1:SSY + 2], ALU.add, (), (b_sc,))
            rsqrt_act(scA[:, SSY:SSY + 1], scA[:, SSY:SSY + 1], 1.0 / D, (), (b_sc,))
            yield
            for half in range(2):
                hs = slice(half * 512, (half + 1) * 512)
                STT(ytmp2, cP[half], scA[:, SSY:SSY + 1], rowb[:, l, RB_POST + half * 512:RB_POST + (half + 1) * 512],
                    ALU.mult, ALU.mult, (b_sc, b_c), (b_cP[half], b_pexp))
                TT("pool", xt[:, hs], xt[:, hs], ytmp2, ALU.add, (b_pexp,), (bx,))
                yield
            if l == DEPTH - 1:
                i = s * NBLK + b
                DMA(out_d[i * 128:(i + 1) * 128, :], xt, d_o[slot], (bx,), ())

        def genQ(s, b, l):
            hT, b_hT = hTs[s], b_hTs[s]
            mixed, bmG = mixeds[s], b_mixG[s]
            S32l, Sbfl, DFtl = S32[:, s, l], Sbf[:, s, l], DFtail[:, s, l]
            if b == 0:
                MEMSET("pool", S32l, 0.0, (b_S32,))
                MEMSET("pool", Sbfl, 0.0, (b_Sbf,))
                MEMSET("pool", DFtl, 0.0, (b_DFt,))
            for c in range(4):
                projF(gA, b_gA, c, 5 + c, l, hT, b_hT)
                yield
            CP("dve", DFw[:, 0:4, 3:131], v3(gA, 4), (), (b_gA, b_DFw))
            projF(gB, b_gB, 0, 9, l, hT, b_hT)
            yield
            projF(gB, b_gB, 1, 10, l, hT, b_hT)
            for kc in range(8):
                MM(gB[:, 256:264], hT[:, kc, :], Wt[:, l, kc, 128:136], (b_hT,), (b_gB,),
                   start=(kc == 0), stop=(kc == 7))
            CP("dve", DFw[:, 4:6, 3:131], v3(gB[:, 0:256], 2), (), (b_gB, b_DFw))
            CP("dve", scG[:, BA:BA + 8], gB[:, 256:264], (), (b_gB, b_scG))
            yield
            CP("pool", DFw[:, :, 0:3], DFtl, (b_DFt,), (b_DFw,))
            sqf = sq[:, :, :].rearrange("p c t -> p (c t)")
            csf = cs[:, 0:4, :].rearrange("p c t -> p (c t)")
            csv = cs[:, 4:6, :].rearrange("p c t -> p (c t)")
            tmpc = gs[:, 3, 256:384]
            gt_b = bc(scG[:, GTOK:GTOK + 4].unsqueeze(2), [128, 4, 128])
            beta_b = bc(scG[:, BETA:BETA + 4].unsqueeze(2), [128, 4, 128])
            egt_b = bc(scG[:, EGT:EGT + 4].unsqueeze(2), [128, 4, 128])
            gr4 = gB.rearrange("p (a b t) -> p a b t", a=2, b=2)

            def y1():
                ACT(scG[:, BETA:BETA + 4], scG[:, BA:BA + 4], AF.Exp, (), (b_scG,), scale=-1.0)
                TT("dve", scG[:, GRAW:GRAW + 4], scG[:, BA + 4:BA + 8], rowb[:, l, RB_DTB:RB_DTB + 4], ALU.add,
                   (b_c,), (b_scG,))

            def y2():
                ACT(scG[:, GRAW:GRAW + 4], scG[:, GRAW:GRAW + 4], AF.Exp, (), (b_scG,))
                TS("dve", scG[:, BETA:BETA + 4], scG[:, BETA:BETA + 4], 1.0, None, ALU.add, None, (), (b_scG,))
                RECIP(scG[:, BETA:BETA + 4], scG[:, BETA:BETA + 4], (), (b_scG,))

            def y3():
                ACT(scG[:, GRAW:GRAW + 4], scG[:, GRAW:GRAW + 4], AF.Ln, (), (b_scG,), bias=1.0)
                TT("dve", scG[:, GRAW:GRAW + 4], scG[:, GRAW:GRAW + 4], lay[:, l, 8:12], ALU.mult, (b_c2,), (b_scG,))

            def y4():
                MM(gB[:, 0:4], utb, scG[:, GRAW:GRAW + 4], (b_scG, b_c), (b_gB,))
                CP("dve", scG[:, GTOK:GTOK + 4], gB[:, 0:4], (), (b_gB, b_scG))

            def y5():
                ACT(scG[:, EGT:EGT + 4], scG[:, GTOK:GTOK + 4], AF.Exp, (), (b_scG,))
                TT("dve", v3(Dt, 4), bc(ident.unsqueeze(1), [128, 4, 128]), gt_b, ALU.mult, (b_c, b_scG), (b_Dt,))

            def y6():
                MM(gB, onesf[:, :], Dt, (b_Dt, b_c2), (b_gB,))
                TT("dve", v3(Dd, 4), gt_b, v3(gB, 4), ALU.subtract, (b_scG,), (b_gB, b_Dd))
                ACT(eG[0:64, :, :], gr4[0:64, :, 0, :], AF.Exp, (), (b_gB, b_eG))
                ACT(eG[64:128, :, :], gr4[64:128, :, 1, :], AF.Exp, (), (b_gB, b_eG))

            def y7():
                TS("dve", Dt, Dd, 0.0, None, ALU.min, None, (b_Dd,), (b_Dt,))
                ACT(E1, Dt, AF.Exp, (b_Dt,), (b_E1,))

            def y8():
                TS("dve", Dt, Dd, -1.0, 0.0, ALU.mult, ALU.min, (b_Dd,), (b_Dt,))
                ACT(E2, Dt, AF.Exp, (b_Dt,), (b_E2,))

            def y9():
                TT("dve", v3(E1, 4), v3(E1, 4), beta_b, ALU.mult, (b_scG,), (b_E1,))

            def y10():
                TT("dve", v3(E1, 4), v3(E1, 4), bc(SLm.unsqueeze(1), [128, 4, 128]), ALU.mult, (b_c,), (b_E1,))

            def y11():
                TT("dve", v3(E2, 4), v3(E2, 4), bc(ULm.unsqueeze(1), [128, 4, 128]), ALU.mult, (b_c,), (b_E2,))

            def xconv(ci):
                def f():
                    cw = PV_CONV + ci * 4
                    TS("dve", cs[:, ci, :], DFw[:, ci, 3:131], pvec[:, l, cw + 3:cw + 4], None, ALU.mult, None,
                       (b_DFw, b_c), (b_cs,))
                    for j in range(3):
                        STT(cs[:, ci, :], DFw[:, ci, j:j + 128], pvec[:, l, cw + j:cw + j + 1], cs[:, ci, :],
                            ALU.mult, ALU.add, (b_DFw, b_c), (b_cs,))
                return f

            def vconv(ci):
                def f():
                    cw = PV_CONV + ci * 4
                    TS("pool", cs[:, ci, :], DFw[:, ci, 3:131], pvec[:, l, cw + 3:cw + 4], 0.0, ALU.mult, ALU.add,
                       (b_DFw, b_c), (b_csv,))
                    for j in range(3):
                        TS("pool", tmpc, DFw[:, ci, j:j + 128], pvec[:, l, cw + j:cw + j + 1], 0.0, ALU.mult, ALU.add,
                           (b_DFw, b_c), (b_tmpv,))
                        TT("pool", cs[:, ci, :], cs[:, ci, :], tmpc, ALU.add, (b_tmpv,), (b_csv,))
                return f

            def x5():
                CP("pool", DFtl, DFw[:, :, 128:131], (b_DFw,), (b_DFt,))
                ACT(sqf, csf, AF.Exp, (b_cs,), (b_sq,), scale=-1.0)

            def x6():
                ACT(sqf, sqf, AF.Ln, (), (b_sq,), bias=1.0)

            def x7():
                ACT(sqf, sqf, AF.Exp, (), (b_sq,), scale=-1.0)

            def x8():
                TT("dve", csf, csf, sqf, ALU.mult, (b_sq,), (b_cs,))

            def x9():
                TT("dve", sqf, csf, csf, ALU.mult, (b_cs,), (b_sq,))
                MM(gA, bones, sqf, (b_sq, b_c), (b_gA,))

            def x10():
                ACT(sqf, gA, AF.Ln, (), (b_gA, b_sq), bias=EPS)

            def x11():
                ACT(sqf, sqf, AF.Exp, (), (b_sq,), scale=-0.5)

            def x12():
                TT("dve", qkn[:, 2:4, :], cs[:, 2:4, :], sq[:, 2:4, :], ALU.mult, (b_cs, b_sq), (b_qkn,))
                STT(qkn[:, 0:2, :], cs[:, 0:2, :], 0.125, sq[:, 0:2, :], ALU.mult, ALU.mult, (b_cs, b_sq), (b_qkn,))

            def x13():
                TT("dve", kTz[:, :, :, :], bc(qkn[:, 2:4, :].unsqueeze(2), [128, 2, 2, 128]),
                   bc(hm.unsqueeze(1).unsqueeze(3), [128, 2, 2, 128]), ALU.mult, (b_qkn, b_c), (b_kTz,))
                for pair in range(2):
                    TR(gT[:, pair * 128:(pair + 1) * 128], qkn[:, 2 + pair, :], idb[:, :], (b_qkn,), (b_gT,))
                CP("act", kvtok[:, 0, :, :].rearrange("p h d -> p (h d)"), gT[:, 0:256], (), (b_gT, b_kvtok))

            def x14():
                for h in range(4):
                    MM(gA[:, h * 128:(h + 1) * 128], kTz[:, h // 2, h % 2, :], qkn[:, 2 + h // 2, :],
                       (b_kTz, b_qkn), (b_gA,))
                for h in range(4):
                    MM(gC[:, h * 128:(h + 1) * 128], kTz[:, h // 2, h % 2, :], qkn[:, h // 2, :],
                       (b_kTz, b_qkn), (b_gC,))

            xs_ = [xconv(0), xconv(1), xconv(2), xconv(3), x5, x6, x7, x8, x9, x10, x11, x12, x13, x14]
            ys_ = [y1, y2, y3, y4, y5, y6, y7, y8, y9, y10, y11]
            vs_ = {0: vconv(4), 1: vconv(5)}
            for n_ in range(max(len(xs_), len(ys_))):
                if n_ < len(ys_):
                    ys_[n_]()
                if n_ < len(xs_):
                    xs_[n_]()
                if n_ in vs_:
                    vs_[n_]()
                yield
            ACT(tmpv, csv, AF.Exp, (b_csv,), (b_tmpv,), scale=-1.0)
            ACT(tmpv, tmpv, AF.Ln, (), (b_tmpv,), bias=1.0)
            ACT(tmpv, tmpv, AF.Exp, (), (b_tmpv,), scale=-1.0)
            TT("dve", Aa[:, 0, :], gA, E1, ALU.mult, (b_E1,), (b_gA, b_A[0]))
            yield
            TT("pool", vT[:, :, :].rearrange("p c t -> p (c t)"), csv, tmpv, ALU.mult, (b_csv, b_tmpv), (b_vT,))
            for h in range(4):
                TR(gT[:, h * 128:(h + 1) * 128], Aa[:, 0, h * 128:(h + 1) * 128], idb[:, :], (b_A[0],), (b_gT,))
            for j2 in range(2):
                cidx = 64 * j2 + 63
                TT("pool", kdz[:, j2, :, :], kvtok[:, 0, :, :],
                   bc(v3(E2, 4)[:, :, cidx:cidx + 1], [128, 4, 64]), ALU.mult, (b_kvtok, b_E2), (b_kdz,))
            yield
            TT("dve", inT, gC, E2, ALU.mult, (b_E2,), (b_gC, b_inT))
            CP("act", Bb[:, 0, :], gT[:, 0:512], (), (b_gT, b_B[0]))
            yield
            TT("dve", v3(Pp[:, 0, :], 4), bc(ident.unsqueeze(1), [128, 4, 128]), v3(gT[:, 0:512], 4),
               ALU.subtract, (b_c,), (b_gT, b_P[0]))
            TT("pool", qg[:, :, :], qkn[:, 0:2, :], eG[:, :, :], ALU.mult, (b_qkn, b_eG), (b_qg,))
            TT("pool", qgz[:, :, :, :], bc(qg[:, :, :].unsqueeze(2), [128, 2, 2, 128]),
               bc(hm.unsqueeze(1).unsqueeze(3), [128, 2, 2, 128]), ALU.mult, (b_qg, b_c), (b_qgz,))
            yield
            for j in range(1, 6):
                c_, n_ = (j - 1) % 2, j % 2
                for h in range(4):
                    hs = slice(h * 128, (h + 1) * 128)
                    MM(gA[:, hs], Bb[:, c_, hs], Aa[:, c_, hs], (b_A[c_], b_B[c_]), (b_gA,))
                if j < 5:
                    for h in range(4):
                        hs = slice(h * 128, (h + 1) * 128)
                        MM(gB[:, hs], Aa[:, c_, hs], Bb[:, c_, hs], (b_A[c_], b_B[c_]), (b_gB,))
                yield
                CP("act", Aa[:, n_, :], gA, (), (b_gA, b_A[n_]))
                if j < 5:
                    CP("dve", Bb[:, n_, :], gB, (), (b_gB, b_B[n_]))
                yield
                for h in range(4):
                    hs = slice(h * 128, (h + 1) * 128)
                    MM(gC[:, hs], Aa[:, n_, hs], Pp[:, c_, hs], (b_A[n_], b_P[c_]), (b_gC,))
                yield
                TT("dve", Pp[:, n_, :], Pp[:, c_, :], gC, ALU.add, (b_P[c_],), (b_gC, b_P[n_]))
                yield
            PT = Pp[:, 1, :]
            for pair in range(2):
                TR(gT[:, 256 + pair * 128:256 + (pair + 1) * 128], vT[:, pair, :], idb[:, :], (b_vT,), (b_gT,))
            CP("act", kvtok[:, 1, :, :].rearrange("p h d -> p (h d)"), gT[:, 256:512], (), (b_gT, b_kvtok))
            TT("dve", v3(Tb, 4), v3(PT, 4), beta_b, ALU.mult, (b_P[1], b_scG), (b_Tb,))
            yield
            TT("dve", v3(Wp, 4), v3(Tb, 4), egt_b, ALU.mult, (b_Tb, b_scG), (b_Wp,))
            for h in range(4):
                MM(gA[:, h * 64:(h + 1) * 64], Tb[:, h * 128:(h + 1) * 128], kvtok[:, 1, h, :],
                   (b_Tb, b_kvtok), (b_gA,))
            yield
            CP("act", uu, gA[:, 0:256], (), (b_gA, b_uo))
            yield
            for j2 in range(2):
                R_ = slice(64 * j2, 64 * j2 + 64)
                ts_ = slice(64 * j2, 64 * j2 + 64)
                for h in range(4):
                    MM(gB[R_, h * 64:(h + 1) * 64], kTz[:, h // 2, h % 2, ts_], Sbfl[:, h // 2, :],
                       (b_kTz, b_Sbf), (b_gB,))
                yield
                CP("act", kS[R_, :], gB[R_, 0:256], (), (b_gB, b_kS))
                yield
                for h in range(4):
                    MM(gA[R_, h * 64:(h + 1) * 64], Wp[:, h * 128 + 64 * j2:h * 128 + 64 * j2 + 64],
                       kS[:, h * 64:(h + 1) * 64], (b_Wp, b_kS), (b_gA,))
                yield
                TT("dve", vnew[R_, :], uu[R_, :], gA[R_, 0:256], ALU.subtract, (b_uo,), (b_gA, b_vnew))
                yield
                for h in range(4):
                    MM(gC[R_, h * 64:(h + 1) * 64], qgz[:, h // 2, h % 2, ts_], Sbfl[:, h // 2, :],
                       (b_qgz, b_Sbf), (b_gC,), start=True, stop=False)
                    MM(gC[R_, h * 64:(h + 1) * 64], inT[:, h * 128 + 64 * j2:h * 128 + 64 * j2 + 64],
                       vnew[:, h * 64:(h + 1) * 64], (b_inT, b_vnew), (b_gC,), start=False, stop=True)
                for h in range(4):
                    h2, pair = h % 2, h // 2
                    MM(gTf[64 * h2:64 * h2 + 64, pair * 64:(pair + 1) * 64], kdz[:, j2, h, :],
                       vnew[:, h * 64:(h + 1) * 64], (b_kdz, b_vnew), (b_gT,))
                yield
                for pair in range(2):
                    STT(S32l[:, pair, :], S32l[:, pair, :], eG[:, pair, 64 * j2 + 63:64 * j2 + 64],
                        gTf[:, pair * 64:(pair + 1) * 64], ALU.mult, ALU.add, (b_eG,), (b_gT, b_S32))
                yield
                CP("dve", Sbfl, S32l, (b_S32,), (b_Sbf,))
                yield
            ACT(osq, gC[:, 0:256], AF.Square, (), (b_gC, b_osq))
            yield
            K.op("dve", lambda e: e.tensor_reduce(scG[:, SS4:SS4 + 4], v3(osq, 4), AX.X, ALU.add),
                 (b_osq,), (b_scG,))
            rsqrt_act(scG[:, SS4:SS4 + 4], scG[:, SS4:SS4 + 4], 1.0 / 64, (), (b_scG,))
            yield
            TT("dve", v3(oo, 4), v3(gC[:, 0:256], 4), bc(scG[:, SS4:SS4 + 4].unsqueeze(2), [128, 4, 64]),
               ALU.mult, (b_scG,), (b_gC, b_uo))
            yield
            TT("pool", v3(mixed[:, 512:768], 4), v3(oo, 4),
               bc(rowb[:, l, RB_DNG:RB_DNG + 64].unsqueeze(1), [128, 4, 64]), ALU.mult,
               (b_uo, b_c), (bmG,))
            yield

        steps = [(s, b, l) for b in range(NBLK) for l in range(DEPTH) for s in range(NSEQ)]
        nst = len(steps)

        def drain(g):
            for _ in g:
                pass

        if NSEQ >= 2:
            drain(genP(*steps[0]))
            for k in range(nst):
                side = []
                if k >= 1:
                    side.append(genR(*steps[k - 1]))
                if k + 1 < nst:
                    side.append(genP(*steps[k + 1]))
                interleave(genQ(*steps[k]), chain(*side), 3, 2)
            drain(genR(*steps[nst - 1]))
        else:
            for st_ in steps:
                drain(genP(*st_))
                drain(genQ(*st_))
                drain(genR(*st_))
        K.final_wait("sp", d_o + [d_dbg])
        K.emit()
    return nc, dbg_d


def _consts():
    c = np.zeros((128, NCONST), np.float32)
    p = np.arange(128)[:, None]
    f = np.arange(128)[None, :]
    same = (p // 64) == (f // 64)
    c[:, C_ID:C_ID + 128] = (p == f)
    c[:, C_SL:C_SL + 128] = same & ((p % 64) > (f % 64))
    c[:, C_UL:C_UL + 128] = same & ((f % 64) >= (p % 64))
    c[:, C_MPREV:C_MPREV + 128] = (p > f)
    c[:, C_MCUR:C_MCUR + 128] = (p <= f)
    c[:, C_BONES:C_BONES + 128] = same
    c[:, C_UTB:C_UTB + 128] = same & ((p % 64) <= (f % 64))
    c[:, C_HM + 0] = (np.arange(128) < 64)
    c[:, C_HM + 1] = (np.arange(128) >= 64)
    return c


def _prep_weights(w_in, w_out, w_mem_kv, pre_norm_g, mem_norm_g, conv_w, a_log, dt_bias, sinks, dn_norm_g,
                  post_norm_g):
    DEPTH = w_in.shape[0]
    qcols = []
    for c in range(4):
        qcols += list(range(O_SQ + c * 64, O_SQ + (c + 1) * 64)) + list(range(O_SQ + (4 + c) * 64, O_SQ + (5 + c) * 64))
    fcols = qcols + list(range(O_SK, O_SK + 128)) + list(range(O_DQ, O_DQ + 768)) + list(range(O_MQ, O_MQ + 256))
    tcols = list(range(O_SV, O_SV + 128)) + list(range(O_BETA, O_BETA + 8)) + list(range(O_GATE, O_GATE + 1024))
    assert len(fcols) == WFC and len(tcols) == WTC
    wall = np.empty((DEPTH, 128, 8, WALLC), np.float32)
    wkv = np.empty((DEPTH, 128, 8, 512), np.float32)
    for l in range(DEPTH):
        wi = w_in[l].reshape(8, 128, -1)
        wall[l, :, :, 0:WFC] = wi[:, :, fcols].transpose(1, 0, 2)
        wall[l, :, :, WFC:WFC + WTC] = wi[:, :, tcols].transpose(1, 0, 2)
        wall[l, :, :, WFC + WTC:] = w_out[l].reshape(8, 128, 1024).transpose(1, 0, 2)
        wkv[l] = w_mem_kv[l].reshape(8, 128, 512).transpose(1, 0, 2)
    pvec = np.zeros((128, DEPTH, NPV), np.float32)
    rowb = np.zeros((128, DEPTH, NRB), np.float32)
    for l in range(DEPTH):
        pvec[:, l, PV_GPRE:PV_GPRE + 8] = pre_norm_g[l].reshape(8, 128).T
        pvec[:, l, PV_GMEM:PV_GMEM + 8] = mem_norm_g[l].reshape(8, 128).T
        pvec[:, l, PV_CONV:PV_CONV + 24] = conv_w[l].reshape(4, 6, 128).transpose(2, 1, 0).reshape(128, 24)
        rowb[:, l, RB_POST:RB_POST + 1024] = post_norm_g[l][None, :]
        rowb[:, l, RB_DNG:RB_DNG + 64] = dn_norm_g[l][None, :]
        rowb[:, l, RB_SINK:RB_SINK + 8] = sinks[l][None, :]
        rowb[:, l, RB_ALOG:RB_ALOG + 4] = a_log[l][None, :]
        rowb[:, l, RB_DTB:RB_DTB + 4] = dt_bias[l][None, :]
    return wall, wkv, pvec, rowb


_NC_CACHE = {}


def run(x, mem, params, ncores, dbg_names=(), self_sync=True, trace=False):
    Bt, S, _ = x.shape
    NSEQ = Bt // ncores
    DEPTH = params["w_in"].shape[0]
    wall, wkv, pvec, rowb = _prep_weights(**params)
    consts = _consts()
    key = (NSEQ, S, DEPTH, tuple(dbg_names), self_sync)
    if key not in _NC_CACHE:
        _NC_CACHE[key] = build(NSEQ, S, DEPTH, dbg_names, self_sync)
    nc, dbg_d = _NC_CACHE[key]
    in_maps = []
    for c in range(ncores):
        in_maps.append({
            "x": np.ascontiguousarray(x[c * NSEQ:(c + 1) * NSEQ].reshape(NSEQ * S, D)),
            "mem": np.ascontiguousarray(mem[c * NSEQ:(c + 1) * NSEQ].reshape(NSEQ * 256, D)),
            "wall": wall, "wkv": wkv, "pvec": pvec, "rowb": rowb, "consts": consts,
        })
    res = run_bass_kernel_spmd(nc, in_maps, core_ids=list(range(ncores)), **({"trace": True} if trace else {}))
    out = np.concatenate([r["out"].reshape(NSEQ, S, D) for r in res.results], axis=0)
    return out, res


def kernel(x, mem, pre_norm_g, w_in, conv_w, a_log, dt_bias, sinks, dn_norm_g, mem_norm_g, w_mem_kv, w_out,
           post_norm_g):
    f = lambda a: np.asarray(a, dtype=np.float32)
    params = dict(w_in=f(w_in), w_out=f(w_out), w_mem_kv=f(w_mem_kv), pre_norm_g=f(pre_norm_g),
                  mem_norm_g=f(mem_norm_g), conv_w=f(conv_w), a_log=f(a_log), dt_bias=f(dt_bias),
                  sinks=f(sinks), dn_norm_g=f(dn_norm_g), post_norm_g=f(post_norm_g))
    out, _ = run(f(x), f(mem), params, NCORES)
    return out.astype(np.float32)
```
